# Optimizing a Trainium2 kernel written in Bass

```python
import jax, jax.numpy as jnp
from jax import lax
import numpy as np

D_MODEL = 1024
BATCH = 2
SEQ = 8192
DEPTH = 1

CHUNK = 64
EPS = 1e-6
D_MIX = D_MODEL
D_A = D_MIX // 2
A_HEADS = 4
A_HEAD_DIM = D_A // A_HEADS
GMLP_BLOCK = 128
D_B = D_MIX - D_A
B_HEADS = 4
B_KEY_DIM = D_B // B_HEADS
B_VAL_DIM = D_B // B_HEADS
D_BK = B_HEADS * B_KEY_DIM
D_BV = B_HEADS * B_VAL_DIM
D_IN = 2 * D_A + 2 * D_BK + 2 * D_BV
N_GROUPS = 4
EXPERTS_PER_GROUP = 8
N_EXPERTS = N_GROUPS * EXPERTS_PER_GROUP
TOP_K_IN_GROUP = 2
D_EXPERT = D_MODEL // 2
MOE_BLOCK = 256

kernel_name = "hybrid_gmlp_hgrn2_hiermoe_block"


def rms_norm(x, gain):
    xf = x.astype(jnp.float32)
    y = xf * lax.rsqrt(jnp.mean(xf * xf, axis=-1, keepdims=True) + EPS)
    return (y * gain.astype(jnp.float32)).astype(x.dtype)


def gmlp_mixer(u, v, v_gain, w_s, b_s):
    bn, s, _ = u.shape
    nb = s // GMLP_BLOCK
    vh = rms_norm(v.reshape(bn, s, A_HEADS, A_HEAD_DIM), v_gain.reshape(A_HEADS, A_HEAD_DIM))
    vh = vh.reshape(bn, nb, GMLP_BLOCK, A_HEADS, A_HEAD_DIM)
    chunk_id = jnp.arange(GMLP_BLOCK) // CHUNK
    mask = chunk_id[None, :] <= chunk_id[:, None]
    w = jnp.where(mask[None], w_s, jnp.zeros_like(w_s))
    mixed = jnp.einsum('gij,bnjgc->bnigc', w, vh) + jnp.transpose(b_s)[:, :, None]
    return u * mixed.reshape(bn, s, D_A).astype(u.dtype)


def hgrn2_mixer(q_raw, f_raw, i_raw, g_raw, lb, out_gain):
    bn, s, _ = q_raw.shape
    n = s // CHUNK
    f32 = jnp.float32
    q = jax.nn.silu(q_raw.astype(f32))
    f = lb + (1.0 - lb) * jax.nn.sigmoid(f_raw.astype(f32))
    k = 1.0 - f
    lf = jnp.log(f)

    def split(t, d):
        return t.reshape(bn, n, CHUNK, B_HEADS, d).transpose(0, 3, 1, 2, 4)

    q, k, lf = split(q, B_KEY_DIM), split(k, B_KEY_DIM), split(lf, B_KEY_DIM)
    v = split(i_raw.astype(f32), B_VAL_DIM)
    b = lax.cumsum(lf, axis=3)
    ref = b[:, :, :, CHUNK // 2 - 1:CHUNK // 2, :]
    qe = q * jnp.exp(b - ref)
    ke = k * jnp.exp(ref - b)
    causal = jnp.tril(jnp.ones((CHUNK, CHUNK), dtype=bool))
    scores = jnp.einsum('bhncd,bhnsd->bhncs', qe, ke)
    scores = jnp.where(causal, scores, 0.0)
    o_intra = jnp.einsum('bhncs,bhnse->bhnce', scores, v)
    b_last = b[:, :, :, -1:, :]
    u_state = jnp.einsum('bhnsd,bhnse->bhnde', k * jnp.exp(b_last - b), v)
    decay = jnp.exp(b_last[:, :, :, 0, :])

    def step(state, xs):
        dec, upd = xs
        return dec[..., None] * state + upd, state

    s0 = jnp.zeros((bn, B_HEADS, B_KEY_DIM, B_VAL_DIM), f32)
    _, s_prev = lax.scan(step, s0, (jnp.moveaxis(decay, 2, 0), jnp.moveaxis(u_state, 2, 0)))
    s_prev = jnp.moveaxis(s_prev, 0, 2)
    o_inter = jnp.einsum('bhncd,bhnde->bhnce', q * jnp.exp(b), s_prev)
    o = (o_intra + o_inter).transpose(0, 2, 3, 1, 4).reshape(bn, s, B_HEADS, B_VAL_DIM)
    o = rms_norm(o, out_gain.reshape(B_HEADS, B_VAL_DIM))
    o = o * jax.nn.silu(g_raw.reshape(bn, s, B_HEADS, B_VAL_DIM).astype(f32))
    return o.reshape(bn, s, D_BV).astype(q_raw.dtype)


def hier_moe(xn, w_gr, b_gr, w_er, b_er, w_gate, w_up, w_down):
    bn, s, d = xn.shape
    t = bn * s
    xt = xn.reshape(t, d)
    f32 = jnp.float32
    g_logits = (xt @ w_gr).astype(f32) + b_gr.astype(f32)
    g_probs = jax.nn.softmax(g_logits, axis=-1)
    g_idx = jnp.argmax(g_logits, axis=-1)
    g_prob = jnp.take_along_axis(g_probs, g_idx[:, None], axis=1)[:, 0]
    e_logits = ((xt @ w_er).astype(f32) + b_er.astype(f32)).reshape(t, N_GROUPS, EXPERTS_PER_GROUP)
    e_logits = jnp.take_along_axis(e_logits, g_idx[:, None, None], axis=1)[:, 0]
    top_v, top_i = lax.top_k(e_logits, TOP_K_IN_GROUP)
    wts = g_prob[:, None] * jax.nn.softmax(top_v, axis=-1)
    eid = (g_idx[:, None] * EXPERTS_PER_GROUP + top_i).reshape(-1).astype(jnp.int32)
    tok = jnp.repeat(jnp.arange(t, dtype=jnp.int32), TOP_K_IN_GROUP)
    wt = wts.reshape(-1)
    n_assign = t * TOP_K_IN_GROUP

    order = jnp.argsort(eid)
    se, stok, swt = eid[order], tok[order], wt[order]
    counts = jnp.bincount(eid, length=N_EXPERTS).astype(jnp.int32)
    padded = ((counts + MOE_BLOCK - 1) // MOE_BLOCK) * MOE_BLOCK
    pend = jnp.cumsum(padded)
    pstart = pend - padded
    start = jnp.cumsum(counts) - counts
    dest = pstart[se] + (jnp.arange(n_assign, dtype=jnp.int32) - start[se])
    n_blk = -(-n_assign // MOE_BLOCK) + N_EXPERTS
    rows = n_blk * MOE_BLOCK
    buf_tok = jnp.full((rows,), t, jnp.int32).at[dest].set(stok)
    buf_wt = jnp.zeros((rows,), f32).at[dest].set(swt)
    blk_expert = jnp.clip(jnp.searchsorted(pend, jnp.arange(n_blk) * MOE_BLOCK, side='right'),
                          0, N_EXPERTS - 1)
    xpad = jnp.concatenate([xt, jnp.zeros((1, d), xt.dtype)], axis=0)

    def expert_block(args):
        tok_b, e = args
        xb = xpad[tok_b]
        h = jax.nn.silu(xb @ w_gate[e]) * (xb @ w_up[e])
        return h @ w_down[e]

    out = lax.map(expert_block, (buf_tok.reshape(n_blk, MOE_BLOCK), blk_expert))
    out = out.reshape(rows, d) * buf_wt[:, None].astype(out.dtype)
    y = jnp.zeros((t + 1, d), out.dtype).at[buf_tok].add(out)[:t]
    return y.reshape(bn, s, d).astype(xn.dtype)


def setup_inputs(seed: int = 0) -> dict:
    key = jax.random.key(seed)
    ks = jax.random.split(key, 20)
    nrm = jax.random.normal
    f32 = jnp.float32
    d = D_MODEL
    return {
        "x": nrm(ks[0], (BATCH, SEQ, d), f32),
        "norm1_gain": 1.0 + 0.05 * nrm(ks[1], (DEPTH, d), f32),
        "w_in": nrm(ks[2], (DEPTH, d, D_IN), f32) * d ** -0.5,
        "gmlp_v_gain": 1.0 + 0.05 * nrm(ks[3], (DEPTH, D_A), f32),
        "gmlp_w_s": nrm(ks[4], (DEPTH, A_HEADS, GMLP_BLOCK, GMLP_BLOCK), f32) * 0.5 * GMLP_BLOCK ** -0.5,
        "gmlp_b_s": 1.0 + 0.1 * nrm(ks[5], (DEPTH, A_HEADS, GMLP_BLOCK), f32),
        "hgrn_lower_bounds": 0.1 * nrm(ks[6], (DEPTH + 1, D_BK), f32),
        "hgrn_out_gain": 1.0 + 0.05 * nrm(ks[7], (DEPTH, D_BV), f32),
        "w_out": nrm(ks[8], (DEPTH, D_MIX, d), f32) * D_MIX ** -0.5,
        "norm2_gain": 1.0 + 0.05 * nrm(ks[9], (DEPTH, d), f32),
        "w_group_router": nrm(ks[10], (DEPTH, d, N_GROUPS), f32) * d ** -0.5,
        "b_group_router": 0.01 * nrm(ks[11], (DEPTH, N_GROUPS), f32),
        "w_expert_router": nrm(ks[12], (DEPTH, d, N_EXPERTS), f32) * d ** -0.5,
        "b_expert_router": 0.01 * nrm(ks[13], (DEPTH, N_EXPERTS), f32),
        "w_gate": nrm(ks[14], (DEPTH, N_EXPERTS, d, D_EXPERT), f32) * d ** -0.5,
        "w_up": nrm(ks[15], (DEPTH, N_EXPERTS, d, D_EXPERT), f32) * d ** -0.5,
        "w_down": nrm(ks[16], (DEPTH, N_EXPERTS, D_EXPERT, d), f32) * D_EXPERT ** -0.5,
        "final_gain": 1.0 + 0.05 * nrm(ks[17], (d,), f32),
    }


def reference(x, norm1_gain, w_in, gmlp_v_gain, gmlp_w_s, gmlp_b_s, hgrn_lower_bounds,
              hgrn_out_gain, w_out, norm2_gain, w_group_router, b_group_router,
              w_expert_router, b_expert_router, w_gate, w_up, w_down, final_gain):
    lb_all = jnp.cumsum(jax.nn.softmax(hgrn_lower_bounds.astype(jnp.float32), axis=0), axis=0)
    split_at = [D_A, 2 * D_A, 2 * D_A + D_BK, 2 * D_A + 2 * D_BK, 2 * D_A + 2 * D_BK + D_BV]
    h = x
    for l in range(DEPTH):
        n1 = rms_norm(h, norm1_gain[l])
        proj = n1 @ w_in[l]
        u, v, q, f, i, g = jnp.split(proj, split_at, axis=-1)
        a_out = gmlp_mixer(jax.nn.gelu(u, approximate=False), jax.nn.gelu(v, approximate=False),
                           gmlp_v_gain[l], gmlp_w_s[l], gmlp_b_s[l])
        b_out = hgrn2_mixer(q, f, i, g, lb_all[l], hgrn_out_gain[l])
        h = h + jnp.concatenate([a_out, b_out], axis=-1) @ w_out[l]
        n2 = rms_norm(h, norm2_gain[l])
        h = h + hier_moe(n2, w_group_router[l], b_group_router[l], w_expert_router[l],
                         b_expert_router[l], w_gate[l], w_up[l], w_down[l])
    return rms_norm(h, final_gain)
```

```python
from contextlib import ExitStack
import os
import numpy as np
import concourse.bass as bass
import concourse.mybir as mybir
from concourse.bass_utils import run_bass_kernel_spmd

F32 = mybir.dt.float32
BF16 = mybir.dt.bfloat16
I32 = mybir.dt.int32
AF = mybir.ActivationFunctionType
ALU = mybir.AluOpType
AX = mybir.AxisListType

NCORES = 8
TOK = 2048
D = 1024
NT = TOK // 128
ST = 256
NST = TOK // ST
NJ = ST // 128
NCH = ST // 64
NPRE = (3 * TOK) // ST
EPS = 1e-6
BLK = 256
NB = (2 * TOK) // BLK + 32
NSLOT = NB * BLK
BIG = 1.0e30
SAME_ENGINE_SYNC = True


_BC_CACHE = {}


def _bc(e, val):
    if val not in _BC_CACHE:
        _BC_CACHE[val] = e.to_reg(val)
    return _BC_CACHE[val]


class Op_:
    __slots__ = ("eng", "fn", "preds", "dma_key", "idx", "level", "sem", "val", "cost")

    def __init__(self, eng, fn, preds, dma_key, idx, cost):
        self.eng = eng
        self.fn = fn
        self.preds = preds
        self.dma_key = dma_key
        self.idx = idx
        self.cost = cost
        self.level = 0.0
        self.sem = None
        self.val = 0


class Buf:
    ALL = []

    def __init__(self, name):
        self.name = name
        self.w = None
        self.r = []
        Buf.ALL.append(self)

    @staticmethod
    def reset_all():
        for b in Buf.ALL:
            b.w = None
            b.r = []


class Sched:
    ENGS = ("sync", "scalar", "vector", "gpsimd", "tensor")
    COST = {"sync": 2.0, "scalar": 1.0, "vector": 1.0, "gpsimd": 1.5, "tensor": 1.0}

    def __init__(self, nc, stack, tag):
        self.nc = nc
        self.stack = stack
        self.tag = tag
        self.ops = []
        self.sem = {e: stack.enter_context(nc.semaphore(f"{tag}_{e}")) for e in self.ENGS}
        self.dsem = {}
        self.final = []
        self.reorder = bool(int(os.environ.get("KREORDER", "1")))

    def _add(self, eng, fn, reads, writes, dma_key, cost):
        preds = set()
        for b in reads:
            if b.w is not None:
                preds.add(b.w)
        for b in writes:
            if b.w is not None:
                preds.add(b.w)
            preds.update(b.r)
        op = Op_(eng, fn, preds, dma_key, len(self.ops), cost)
        lv = 0.0
        for p in preds:
            lv = max(lv, p.level + p.cost)
        op.level = lv
        self.ops.append(op)
        for b in reads:
            b.r.append(op)
        for b in writes:
            b.w = op
            b.r = []
        return op

    def op(self, eng, fn, reads=(), writes=(), cost=None):
        return self._add(eng, fn, reads, writes, None, self.COST[eng] if cost is None else cost)

    def dma(self, eng, fn, reads=(), writes=(), key=None, final=False, cost=None):
        if key is None:
            key = writes[0].name if writes else reads[0].name
        if key not in self.dsem:
            self.dsem[key] = self.stack.enter_context(self.nc.semaphore(f"{self.tag}_d{len(self.dsem)}"))
        return self._add(eng, fn, reads, writes, key, 3.0 if cost is None else cost)

    def emit(self, block):
        order = sorted(self.ops, key=lambda o: (o.level, o.idx)) if self.reorder else list(self.ops)
        cnt = {e: 0 for e in self.ENGS}
        dcnt = {k: 0 for k in self.dsem}
        q = {e: [] for e in self.ENGS}
        waited = {e: {} for e in self.ENGS}
        for o in order:
            waits = {}
            wd = waited[o.eng]
            for p in o.preds:
                assert p.sem is not None, "predecessor not scheduled before successor"
                if p.dma_key is None and p.eng == o.eng and (o.eng == "tensor" or not SAME_ENGINE_SYNC):
                    continue
                k = id(p.sem)
                if wd.get(k, 0) < p.val:
                    if k not in waits or waits[k][1] < p.val:
                        waits[k] = (p.sem, p.val)
            for k, (sm, v) in waits.items():
                wd[k] = v
            if o.dma_key is None:
                cnt[o.eng] += 1
                o.sem, o.val = self.sem[o.eng], cnt[o.eng]
                inc = (o.sem, 1)
            else:
                dcnt[o.dma_key] += 16
                o.sem, o.val = self.dsem[o.dma_key], dcnt[o.dma_key]
                inc = (o.sem, 16)
            q[o.eng].append((list(waits.values()), o.fn, inc))
        fin = [(self.dsem[k], dcnt[k]) for k in self.dsem if dcnt[k] > 0]

        def run(eng, e):
            for waits, fn, inc in q[eng]:
                for sm, v in waits:
                    e.wait_ge(sm, v)
                ins = fn(e)
                ins.then_inc(inc[0], inc[1])
            if eng == "sync":
                for sm, v in fin:
                    e.wait_ge(sm, v)

        @block.sync
        def _(e):
            run("sync", e)

        @block.scalar
        def _(e):
            run("scalar", e)

        @block.vector
        def _(e):
            run("vector", e)

        @block.gpsimd
        def _(e):
            run("gpsimd", e)

        @block.tensor
        def _(e):
            run("tensor", e)


def build(debug=False, phases=2):
    _BC_CACHE.clear()
    nc = bass.Bass("TRN2", target_bir_lowering=False)
    dt = nc.dram_tensor
    x_d = dt("x", [TOK, D], F32, kind="ExternalInput").ap()
    xpre_d = dt("xpre", [NPRE * ST, D], F32, kind="ExternalInput").ap()
    win_d = dt("win", [128, 8, 3072], F32, kind="ExternalInput").ap()
    wout_d = dt("wout", [128, 8, 1024], F32, kind="ExternalInput").ap()
    wsT_d = dt("wsT", [128, 4, 128], F32, kind="ExternalInput").ap()
    bsbc_d = dt("bsbc", [128, 4, 128], F32, kind="ExternalInput").ap()
    small_d = dt("small", [128, 32], F32, kind="ExternalInput").ap()
    g2bc_d = dt("g2bc", [128, D], F32, kind="ExternalInput").ap()
    gfbc_d = dt("gfbc", [128, D], F32, kind="ExternalInput").ap()
    wr_d = dt("wr", [128, 8, 36], F32, kind="ExternalInput").ap()
    brbc_d = dt("brbc", [128, 36], F32, kind="ExternalInput").ap()
    consts_d = dt("consts", [128, 5, 128], F32, kind="ExternalInput").ap()
    rmask_d = dt("rmask", [128, 1024], F32, kind="ExternalInput").ap()
    blkthr_d = dt("blkthr", [128, NB], F32, kind="ExternalInput").ap()
    if phases >= 2:
        wg_d = dt("wg", [32 * 128, 4096], F32, kind="ExternalInput").ap()
        wu_d = dt("wu", [32 * 128, 4096], F32, kind="ExternalInput").ap()
        wd_d = dt("wd", [32 * 128, 4096], F32, kind="ExternalInput").ap()
    out_d = dt("out", [TOK, D], F32, kind="ExternalOutput").ap()
    hs_d = dt("hs", [TOK, D], F32, kind="Internal").ap()
    n2s_d = dt("n2s", [TOK, D], BF16, kind="Internal").ap()
    xs_d = dt("xs", [NSLOT, D], BF16, kind="Internal").ap()
    ys_d = dt("ys", [NSLOT, D], F32, kind="Internal").ap()
    if phases >= 2:
        wgb_d = dt("wgb", [32 * 128, 4096], BF16, kind="Internal").ap()
        wub_d = dt("wub", [32 * 128, 4096], BF16, kind="Internal").ap()
        wdb_d = dt("wdb", [32 * 128, 4096], BF16, kind="Internal").ap()
    if debug:
        dbg_h = dt("dbg_h", [TOK, D], F32, kind="ExternalOutput").ap()
        dbg_lg = dt("dbg_lg", [128, NT, 36], F32, kind="ExternalOutput").ap()
        dbg_s = dt("dbg_s", [128, 4, 128], F32, kind="ExternalOutput").ap()
        dbg_dec = dt("dbg_dec", [128, 4, NCH], F32, kind="ExternalOutput").ap()
        dbg_r = dt("dbg_r", [128, 8, NT], F32, kind="ExternalOutput").ap()

    with ExitStack() as gs:
        def sb(stack, name, shape, dtype):
            return stack.enter_context(nc.sbuf_tensor("s_" + name, shape, dtype))

        def ps(stack, name, shape, dtype):
            return stack.enter_context(nc.psum_tensor("p_" + name, shape, dtype))

        lg = sb(gs, "lg", [128, NT, 36], F32)
        cst = sb(gs, "cst", [128, 5, 128], F32)
        small = sb(gs, "small", [128, 32], F32)
        epsb = sb(gs, "epsb", [128, 1], F32)
        oneb = sb(gs, "oneb", [128, 1], F32)

        with ExitStack() as s1:
            S = Sched(nc, s1, "a")
            B = Buf
            win_bf = sb(s1, "win_bf", [128, 8, 3072], BF16)
            wout_bf = sb(s1, "wout_bf", [128, 8, 1024], BF16)
            wmT = sb(s1, "wmT", [128, 4, 128], BF16)
            bsbc = sb(s1, "bsbc", [128, 4, 128], F32)
            g2bc = sb(s1, "g2bc", [128, D], F32)
            wr = sb(s1, "wr", [128, 8, 36], F32)
            brbc = sb(s1, "brbc", [128, 36], F32)
            rmask = sb(s1, "rmask", [128, 4 * ST], F32)
            identb = sb(s1, "identb", [128, 128], BF16)
            onesb = sb(s1, "onesb", [128, 128], BF16)
            lbt = sb(s1, "lbt", [128, 12], F32)
            NXS = 4
            xt = [sb(s1, f"xt{i}", [128, D], F32) for i in range(NXS)]
            sqj = sb(s1, "sqj", [128, D], BF16)
            stat = sb(s1, "stat", [128, NT + 2, 8], F32)
            xn = [sb(s1, f"xn{i}", [128, D], BF16) for i in range(2)]
            n1T = [sb(s1, f"n1T{i}", [128, 8, ST], BF16) for i in range(2)]
            guT = sb(s1, "guT", [128, 4, ST], BF16)
            sgT = sb(s1, "sgT", [128, 4, ST], BF16)
            dec = sb(s1, "dec", [128, 4, NCH], F32)
            qeT = sb(s1, "qeT", [128, 4, ST], BF16)
            keT = sb(s1, "keT", [128, 4, ST], BF16)
            qdT = sb(s1, "qdT", [128, 4, ST], BF16)
            kdtm = [sb(s1, f"kdtm{i}", [128, 4, NJ, 128], BF16) for i in range(2)]
            gv = sb(s1, "gv", [128, 512], F32)
            gsq = sb(s1, "gsq", [128, 512], F32)
            vst = sb(s1, "vst", [128, NJ, 12], F32)
            vhn = sb(s1, "vhn", [128, NJ, 512], BF16)
            vi = sb(s1, "vi", [128, NJ, 512], BF16)
            tmpA = [sb(s1, f"tmpA{i}", [128, 128], F32) for i in range(2)]
            S32 = sb(s1, "S32", [128, 4, 128], F32)
            Sbf4 = [sb(s1, f"Sbf4_{i}", [128, 4, 128], BF16) for i in range(3)]
            n2b = sb(s1, "n2b", [128, D], BF16)
            pT = ps(s1, "pT", [128, 1024], BF16)
            pK = ps(s1, "pK", [128, 1024], BF16)
            pP = [ps(s1, f"pP{i}", [128, 512], F32) for i in range(2)]
            pG = ps(s1, "pG", [128, 512], F32)
            pS = ps(s1, "pS", [128, 512], F32)
            pO = ps(s1, "pO", [128, 512], F32)
            pU = ps(s1, "pU", [128, 512], F32)

            b_cst = B("cst"); b_small = B("small"); b_lg = B("lg")
            b_win = [B(f"win{k}") for k in range(8)]
            b_wout = B("wout")
            b_wstage = B("wstage")
            b_wsT = B("wsT"); b_wmT = B("wmT"); b_bsbc = B("bsbc"); b_g2bc = B("g2bc")
            b_wr = B("wr"); b_brbc = B("brbc"); b_rmask = B("rmask")
            b_identb = B("identb"); b_onesb = B("onesb"); b_lbt = B("lbt")
            b_xt = [B(f"xt{i}") for i in range(NXS)]
            b_sqj = B("sqj")
            b_stat = [B(f"stat{t}") for t in range(NT + 2)]
            b_xn = [B(f"xn{i}") for i in range(2)]
            b_n1T = [[B(f"n1T{i}_{j}") for j in range(NJ)] for i in range(2)]
            b_guT = [B(f"guT{g}") for g in range(4)]
            b_sgT = [B(f"sgT{g}") for g in range(4)]
            b_qs = [B(f"qs{i}") for i in range(2)]
            b_fk = [B(f"fk{i}") for i in range(2)]
            b_l1 = [B(f"l1{i}") for i in range(2)]
            b_bT = [B(f"bT{i}") for i in range(2)]
            b_d3 = [B(f"d3{i}") for i in range(2)]
            b_E12 = [B(f"E12{i}") for i in range(2)]
            b_kdh = [B(f"kdh{i}") for i in range(2)]
            b_dec = [B(f"dec{h}") for h in range(4)]
            b_qeT = [B(f"qeT{h}") for h in range(4)]
            b_keT = [B(f"keT{h}") for h in range(4)]
            b_qdT = [B(f"qdT{h}") for h in range(4)]
            b_kdtm = [B(f"kdtm{h}") for h in range(4)]
            b_gv = B("gv")
            b_gsq = B("gsq")
            b_vst = [B(f"vst{j}") for j in range(NJ)]
            b_vhn = [B(f"vhn{j}") for j in range(NJ)]
            b_vi = [B(f"vi{j}") for j in range(NJ)]
            b_tmpA = [B(f"tmpA{i}") for i in range(2)]
            b_scb = [B(f"scb{i}") for i in range(4)]
            b_S32 = [B(f"S32{h}") for h in range(4)]
            b_Sbf4 = [B(f"Sbf4_{i}") for i in range(3)]
            b_o32 = [B(f"o32{h}") for h in range(4)]
            b_osq = B("osq"); b_osd = B("osd"); b_ors = B("ors"); b_otmp = B("otmp")
            b_abT = [[B(f"abT{c}_{j}") for j in range(NJ)] for c in range(8)]
            b_hx = [B(f"hx{i}") for i in range(2)]
            b_n2f = B("n2f")
            b_n2b = B("n2b")
            b_n2T = B("n2T")
            b_pT = B("pT"); b_pK = [B("pK0"), B("pK1")]
            b_pP = [B("pP0"), B("pP1")]
            b_pG = B("pG")
            b_pS = [B(f"pS{i}") for i in range(4)]
            b_pO = [B(f"pO{i}") for i in range(4)]
            b_pU = [B(f"pU{i}") for i in range(4)]
            b_hs = B("hs_d"); b_n2s = B("n2s_d")
            b_dbg = B("dbg")

            s1a = ExitStack()
            wstage = sb(s1a, "wstage", [128, 3072], F32)
            wsT = sb(s1a, "wsT", [128, 4, 128], F32)
            sg4 = [sb(s1a, f"sg4{i}", [128, 4, ST], F32) for i in range(2)]
            vip = [sb(s1a, f"vip{i}", [128, NJ, 512], BF16) for i in range(2)]
            l14 = sb(s1a, "l14", [128, 4, ST], F32)
            k4 = sb(s1a, "k4", [128, 4, ST], F32)
            bT4 = sb(s1a, "bT4", [128, 4, ST], F32)
            d34 = sb(s1a, "d34", [128, 4, ST], F32)
            kd4 = sb(s1a, "kd4", [128, 4, ST], BF16)
            blp = sb(s1a, "blp", [128, 4, NCH], F32)
            decp = sb(s1a, "decp", [128, 4, NCH], F32)
            b_sg4 = [B("sg40"), B("sg41")]; b_vip = [B("vip0"), B("vip1")]
            b_l14 = B("l14"); b_k4 = B("k4"); b_bT4 = B("bT4"); b_d34 = B("d34"); b_kd4 = B("kd4")
            b_blp = B("blp"); b_decp = B("decp")
            S.dma("sync", lambda e: e.dma_start(out=cst[:], in_=consts_d[:, :, :]), writes=[b_cst])
            S.dma("sync", lambda e: e.dma_start(out=small[:], in_=small_d[:, :]), writes=[b_small])
            S.dma("sync", lambda e: e.dma_start(out=wsT[:], in_=wsT_d[:, :, :]), writes=[b_wsT])
            S.dma("sync", lambda e: e.dma_start(out=bsbc[:], in_=bsbc_d[:, :, :]), writes=[b_bsbc])
            S.dma("sync", lambda e: e.dma_start(out=rmask[:], in_=rmask_d[:, 0:4 * ST]), writes=[b_rmask])
            S.dma("scalar", lambda e: e.dma_start(out=g2bc[:], in_=g2bc_d[:, :]), writes=[b_g2bc])
            S.dma("scalar", lambda e: e.dma_start(out=wr[:], in_=wr_d[:, :, :]), writes=[b_wr])
            S.dma("scalar", lambda e: e.dma_start(out=brbc[:], in_=brbc_d[:, :]), writes=[b_brbc])
            for k in range(8):
                S.dma("gpsimd", lambda e, k=k: e.dma_start(out=wout_bf[:, k, :], in_=wout_d[:, k, :]),
                      writes=[b_wout], key="wout")
            b_epsb = B("epsb")
            S.op("vector", lambda e: e.memset(epsb[:], EPS), writes=[b_epsb])
            S.op("vector", lambda e: e.memset(oneb[:], 1.0), writes=[b_epsb])
            S.op("vector", lambda e: e.tensor_copy(out=identb[:], in_=cst[:, 0, :]), reads=[b_cst], writes=[b_identb])
            S.op("vector", lambda e: e.tensor_copy(out=onesb[:], in_=cst[:, 4, :]), reads=[b_cst], writes=[b_onesb])
            S.op("vector", lambda e: e.tensor_tensor(out=wmT[:], in0=wsT[:], in1=cst[:, 2:3, :].to_broadcast([128, 4, 128]),
                                                     op=ALU.mult), reads=[b_cst, b_wsT], writes=[b_wmT])
            lbraw = small[:, 4:12].rearrange("p (h r) -> p h r", r=2)
            S.op("vector", lambda e: e.tensor_tensor(out=lbt[:, 8:12], in0=lbraw[:, :, 0], in1=lbraw[:, :, 1], op=ALU.subtract),
                 reads=[b_small], writes=[b_lbt])
            S.op("scalar", lambda e: e.activation(out=lbt[:, 0:4], in_=lbt[:, 8:12], func=AF.Sigmoid),
                 reads=[b_lbt], writes=[b_lbt])
            S.op("vector", lambda e: e.tensor_scalar(out=lbt[:, 4:8], in0=lbt[:, 0:4], scalar1=-1.0, scalar2=1.0,
                                                     op0=ALU.mult, op1=ALU.add), reads=[b_lbt], writes=[b_lbt])
            S.op("vector", lambda e: e.tensor_scalar(out=lbt[:, 8:12], in0=lbt[:, 4:8], scalar1=-1.0, scalar2=None, op0=ALU.mult),
                 reads=[b_lbt], writes=[b_lbt])
            S.op("vector", lambda e: e.memset(S32[:], 0.0), writes=b_S32)
            S.op("vector", lambda e: e.memset(kdtm[0][:], 0.0), writes=b_kdtm)
            S.op("vector", lambda e: e.memset(kdtm[1][:], 0.0), writes=b_kdtm)
            S.op("vector", lambda e: e.memset(Sbf4[0][:], 0.0), writes=[b_Sbf4[0]])
            for k in range(8):
                S.dma("sync" if k % 2 == 0 else "scalar",
                      lambda e, k=k: e.dma_start(out=wstage[:], in_=win_d[:, k, :]), writes=[b_wstage])
                S.op("scalar", lambda e, k=k: e.activation(out=win_bf[:, k, 0:1536], in_=wstage[:, 0:1536], func=AF.Copy,
                                                           scale=small[:, 16 + k:17 + k]),
                     reads=[b_wstage, b_small], writes=[b_win[k]])
                S.op("vector", lambda e, k=k: e.tensor_scalar(out=win_bf[:, k, 1536:3072], in0=wstage[:, 1536:3072],
                                                              scalar1=small[:, 16 + k:17 + k], scalar2=None, op0=ALU.mult),
                     reads=[b_wstage, b_small], writes=[b_win[k]])

            sbf_cur = [0, 0, 0, 0]
            precast = []
            if phases >= 2:
                for ex in range(32):
                    for src, dst in ((wg_d, wgb_d), (wu_d, wub_d), (wd_d, wdb_d)):
                        precast.append((src, dst, ex))

            def emit_precast(n, dep):
                for _ in range(n):
                    if not precast:
                        return
                    src, dst, ex = precast.pop(0)
                    S.dma("gpsimd", lambda e, src=src, dst=dst, ex=ex: e.dma_start(
                        out=dst[ex * 128:(ex + 1) * 128, :].rearrange("r (a c) -> r a c", c=2048),
                        in_=src[ex * 128:(ex + 1) * 128, :].rearrange("r (a c) -> r a c", c=2048)),
                        reads=[dep], writes=[B(f"pc{len(precast)}")], key=f"pc{len(precast) % 4}")

            def rms_stats(tile_t, src, b_src, col, junk=None, b_junk=None):
                if junk is None:
                    junk, b_junk = sqj, b_sqj
                S.op("scalar", lambda e: e.activation(out=junk[:], in_=src, func=AF.Square,
                                                      accum_out=stat[:, tile_t, col:col + 1]),
                     reads=[b_src], writes=[b_junk, b_stat[tile_t]])
                S.op("scalar", lambda e: e.activation(out=stat[:, tile_t, col + 1:col + 2], in_=stat[:, tile_t, col:col + 1],
                                                      func=AF.Ln, scale=1.0 / D, bias=epsb[:, 0:1]),
                     reads=[b_stat[tile_t], b_epsb], writes=[b_stat[tile_t]])
                S.op("scalar", lambda e: e.activation(out=stat[:, tile_t, col + 2:col + 3], in_=stat[:, tile_t, col + 1:col + 2],
                                                      func=AF.Exp, scale=-0.5),
                     reads=[b_stat[tile_t]], writes=[b_stat[tile_t]])

            pp_i = [0]

            def next_pp():
                i = pp_i[0]
                pp_i[0] ^= 1
                return i

            gcount = [0]

            def super_tile(st, pre, last_pre=False):
                nb = gcount[0] % 2
                gcount[0] += 1
                src_d = xpre_d if pre else x_d
                abT = abT2[st % 2]
                b_abT = b_abT2[st % 2]
                sgT_ = sgT2[st % 2]
                b_sgT_ = b_sgT2[st % 2]
                for j in range(NJ):
                    t = st * NJ + j
                    xs_i = t % NXS
                    S.dma("sync", lambda e, t=t, xs_i=xs_i: e.dma_start(out=xt[xs_i][:], in_=src_d[t * 128:(t + 1) * 128, :]),
                          writes=[b_xt[xs_i]])
                    ts = (NT + t % 2) if pre else t
                    rms_stats(ts, xt[xs_i][:], b_xt[xs_i], 0)
                    xi = t % 2
                    S.op("vector", lambda e, ts=ts, xs_i=xs_i, xi=xi: e.tensor_scalar(out=xn[xi][:], in0=xt[xs_i][:],
                                                                                     scalar1=stat[:, ts, 2:3], scalar2=None,
                                                                                     op0=ALU.mult),
                         reads=[b_xt[xs_i], b_stat[ts]], writes=[b_xn[xi]])

                    def tr(e, xi=xi):
                        ins = None
                        for k in range(8):
                            ins = e.transpose(out=pT[:, k * 128:(k + 1) * 128], in_=xn[xi][:, k * 128:(k + 1) * 128],
                                              identity=identb[:])
                        return ins
                    S.op("tensor", tr, reads=[b_xn[xi], b_identb], writes=[b_pT])
                    S.op("vector", lambda e, nb=nb, j=j: e.tensor_copy(
                        out=n1T[nb][:, :, j * 128:(j + 1) * 128], in_=pT[:].rearrange("p (k t) -> p k t", t=128)),
                        reads=[b_pT], writes=[b_n1T[nb][j]])

                def proj_fm(col0, pi, nb=nb):
                    def f(e):
                        ins = None
                        for k in range(8):
                            ins = e.matmul(pP[pi][:, 0:ST], lhsT=win_bf[:, k, col0:col0 + 128], rhs=n1T[nb][:, k, :],
                                           start=(k == 0), stop=(k == 7))
                        return ins
                    S.op("tensor", f, reads=b_win + b_n1T[nb], writes=[b_pP[pi]])

                def proj_tm(col0, j, pi, nb=nb):
                    def f(e):
                        ins = None
                        for k in range(8):
                            ins = e.matmul(pP[pi][:], lhsT=n1T[nb][:, k, j * 128:(j + 1) * 128], rhs=win_bf[:, k, col0:col0 + 512],
                                           start=(k == 0), stop=(k == 7))
                        return ins
                    S.op("tensor", f, reads=b_win + [b_n1T[nb][j]], writes=[b_pP[pi]])

                KPCUT = int(os.environ.get("KPCUT", "9"))
                if pre and KPCUT <= 0:
                    return
                if not pre:
                    emit_precast(8 - (st % 2), b_n1T[nb][0])
                for g in range(0 if pre else 4):
                    pi = next_pp()
                    proj_fm(g * 128, pi)
                    S.op("scalar", lambda e, g=g, pi=pi: e.activation(out=guT[:, g, :], in_=pP[pi][:, 0:ST], func=AF.Gelu),
                         reads=[b_pP[pi]], writes=[b_guT[g]])
                for j in range(NJ):
                    if pre:
                        pi = next_pp()
                        proj_tm(2048, j, pi)
                        S.op("scalar", lambda e, pi=pi, j=j: e.activation(out=vi[:, j, :], in_=pP[pi][:], func=AF.Copy),
                             reads=[b_pP[pi]], writes=[b_vi[j]])
                        continue
                    pi = next_pp()
                    proj_tm(512, j, pi)
                    S.op("scalar", lambda e, pi=pi: e.activation(out=gv[:], in_=pP[pi][:], func=AF.Gelu),
                         reads=[b_pP[pi]], writes=[b_gv])
                    S.op("vector", lambda e: e.tensor_tensor(out=gsq[:], in0=gv[:], in1=gv[:], op=ALU.mult),
                         reads=[b_gv], writes=[b_gsq])
                    S.op("vector", lambda e, j=j: e.tensor_reduce(out=vst[:, j, 0:4], in_=gsq[:].rearrange("p (g c) -> p g c", c=128),
                                                                 axis=AX.X, op=ALU.add),
                         reads=[b_gsq], writes=[b_vst[j]])
                    S.op("scalar", lambda e, j=j: e.activation(out=vst[:, j, 4:8], in_=vst[:, j, 0:4], func=AF.Ln,
                                                               scale=1.0 / 128, bias=epsb[:, 0:1]),
                         reads=[b_vst[j], b_epsb], writes=[b_vst[j]])
                    S.op("scalar", lambda e, j=j: e.activation(out=vst[:, j, 8:12], in_=vst[:, j, 4:8], func=AF.Exp, scale=-0.5),
                         reads=[b_vst[j]], writes=[b_vst[j]])
                    S.op("vector", lambda e, j=j: e.tensor_tensor(
                        out=vhn[:, j, :].rearrange("p (g c) -> p g c", c=128),
                        in0=gv[:].rearrange("p (g c) -> p g c", c=128),
                        in1=vst[:, j, 8:12].rearrange("p (g o) -> p g o", o=1).to_broadcast([128, 4, 128]), op=ALU.mult),
                        reads=[b_gv, b_vst[j]], writes=[b_vhn[j]])
                    pi = next_pp()
                    proj_tm(2048, j, pi)
                    S.op("scalar", lambda e, pi=pi, j=j: e.activation(out=vi[:, j, :], in_=pP[pi][:], func=AF.Copy),
                         reads=[b_pP[pi]], writes=[b_vi[j]])
                if pre and KPCUT <= 1:
                    return
                for j in range(0 if pre else NJ):
                    def gm(e, j=j):
                        ins = None
                        for g in range(4):
                            ins = e.matmul(pG[:, g * 128:(g + 1) * 128], lhsT=vhn[:, j, g * 128:(g + 1) * 128], rhs=wmT[:, g, :],
                                           start=True, stop=True)
                        return ins
                    S.op("tensor", gm, reads=[b_vhn[j], b_wmT], writes=[b_pG])
                    for g in range(4):
                        ti = g % 2
                        S.op("vector", lambda e, g=g, ti=ti: e.scalar_tensor_tensor(
                            out=tmpA[ti][:], in0=pG[:, g * 128:(g + 1) * 128], scalar=small[:, g:g + 1], in1=bsbc[:, g, :],
                            op0=ALU.mult, op1=ALU.add), reads=[b_pG, b_small, b_bsbc], writes=[b_tmpA[ti]])
                        S.op("gpsimd", lambda e, g=g, ti=ti, j=j: e.tensor_tensor(
                            out=abT[:, g, j * 128:(j + 1) * 128], in0=tmpA[ti][:], in1=guT[:, g, j * 128:(j + 1) * 128], op=ALU.mult),
                            reads=[b_tmpA[ti], b_guT[g]], writes=[b_abT[g][j]])
                def proj2(col_base, bank, b_bank, bk, nb=nb):
                    def f(e):
                        ins = None
                        for hh in range(2):
                            h = bk * 2 + hh
                            for k in range(8):
                                ins = e.matmul(bank[:, hh * ST:(hh + 1) * ST], lhsT=win_bf[:, k, col_base + h * 128:col_base + (h + 1) * 128],
                                               rhs=n1T[nb][:, k, :], start=(k == 0), stop=(k == 7))
                        return ins
                    S.op("tensor", f, reads=b_win + b_n1T[nb], writes=b_bank)

                def v2(bank):
                    return bank[:, 0:2 * ST].rearrange("p (h t) -> p h t", t=ST)
                fbanks = ((pP[0], [b_pP[0]]), (pP[1], [b_pP[1]]))
                qbanks = fbanks
                gbanks = fbanks
                for bk in range(2):
                    bank, bb = fbanks[bk]
                    proj2(1536, bank, bb, bk)
                    S.op("scalar", lambda e, bk=bk, bank=bank: e.activation(out=sg4m[:, bk * 2:(bk + 1) * 2, :], in_=v2(bank), func=AF.Exp,
                                                                          scale=-1.0), reads=bb, writes=[b_sg4m])
                S.op("scalar", lambda e: e.activation(out=sg4m[:], in_=sg4m[:], func=AF.Ln, bias=oneb[:, 0:1]),
                     reads=[b_sg4m], writes=[b_sg4m])
                S.op("scalar", lambda e: e.activation(out=sg4m[:], in_=sg4m[:], func=AF.Exp, scale=-1.0),
                     reads=[b_sg4m], writes=[b_sg4m])
                for bk in range(2):
                    bank, bb = qbanks[bk]
                    proj2(1024, bank, bb, bk)
                    S.op("scalar", lambda e, bk=bk, bank=bank: e.activation(out=qs4[:, bk * 2:(bk + 1) * 2, :], in_=v2(bank), func=AF.Silu),
                         reads=bb, writes=[b_qs4])
                for bk in range(2):
                    bank, bb = gbanks[bk]
                    proj2(2560, bank, bb, bk)
                    S.op("scalar", lambda e, bk=bk, bank=bank: e.activation(out=sgT_[:, bk * 2:(bk + 1) * 2, :], in_=v2(bank), func=AF.Silu),
                         reads=bb, writes=[b_sgT_[bk * 2], b_sgT_[bk * 2 + 1]])
                for h in range(4):
                    S.op("scalar", lambda e, h=h: e.activation(out=l14m[:, h, :], in_=sg4m[:, h, :], func=AF.Ln,
                                                               scale=lbt[:, 4 + h:5 + h], bias=lbt[:, h:h + 1]),
                         reads=[b_sg4m, b_lbt], writes=[b_l14m])
                for h in range(4):
                    S.op("vector", lambda e, h=h: e.tensor_scalar(
                        out=k4m[:, h, :], in0=sg4m[:, h, :], scalar1=lbt[:, 8 + h:9 + h], scalar2=lbt[:, 4 + h:5 + h],
                        op0=ALU.mult, op1=ALU.add), reads=[b_sg4m, b_lbt], writes=[b_k4m])
                S.op("vector", lambda e: e.tensor_tensor_scan(out=bT4m[:].rearrange("p h t -> p (h t)"), data0=rmask[:],
                                                              data1=l14m[:].rearrange("p h t -> p (h t)"), initial=0.0,
                                                              op0=ALU.mult, op1=ALU.add),
                     reads=[b_l14m, b_rmask], writes=[b_bT4m])
                bvm = bT4m[:].rearrange("p h (c t) -> p (h c) t", t=64)
                S.op("vector", lambda e: e.tensor_tensor(
                    out=l14m[:].rearrange("p h (c t) -> p (h c) t", t=64), in0=bvm,
                    in1=bvm[:, :, 31:32].to_broadcast([128, 4 * NCH, 64]), op=ALU.subtract),
                    reads=[b_bT4m], writes=[b_l14m])
                S.op("vector", lambda e: e.tensor_tensor(
                    out=d34m[:].rearrange("p h (c t) -> p (h c) t", t=64), in0=bvm,
                    in1=bvm[:, :, 63:64].to_broadcast([128, 4 * NCH, 64]), op=ALU.subtract),
                    reads=[b_bT4m], writes=[b_d34m])
                S.op("vector", lambda e: e.tensor_copy(out=blm[:].rearrange("p h c -> p (h c)"), in_=bvm[:, :, 63]),
                     reads=[b_bT4m], writes=[b_blm])
                S.op("scalar", lambda e: e.activation(out=E12m[:], in_=l14m[:], func=AF.Exp), reads=[b_l14m], writes=[b_E12m])
                S.op("vector", lambda e: e.tensor_tensor(out=qeT[:], in0=qs4[:], in1=E12m[:], op=ALU.mult),
                     reads=[b_qs4, b_E12m], writes=b_qeT)
                S.op("scalar", lambda e: e.activation(out=E12m[:], in_=l14m[:], func=AF.Exp, scale=-1.0), reads=[b_l14m], writes=[b_E12m])
                S.op("vector", lambda e: e.tensor_tensor(out=keT[:], in0=k4m[:], in1=E12m[:], op=ALU.mult),
                     reads=[b_k4m, b_E12m], writes=b_keT)
                S.op("scalar", lambda e: e.activation(out=d34m[:], in_=d34m[:], func=AF.Exp, scale=-1.0), reads=[b_d34m], writes=[b_d34m])
                S.op("gpsimd", lambda e: e.tensor_tensor(out=kd4m[:], in0=k4m[:], in1=d34m[:], op=ALU.mult),
                     reads=[b_k4m, b_d34m], writes=[b_kd4m])
                S.op("scalar", lambda e: e.activation(out=decm[:], in_=blm[:], func=AF.Exp), reads=[b_blm], writes=[b_decm])
                S.op("scalar", lambda e: e.activation(out=bT4m[:], in_=bT4m[:], func=AF.Exp), reads=[b_bT4m], writes=[b_bT4m])
                S.op("vector", lambda e: e.tensor_tensor(out=qdT[:], in0=qs4[:], in1=bT4m[:], op=ALU.mult),
                     reads=[b_qs4, b_bT4m], writes=b_qdT)

                def trkm(e):
                    ins = None
                    for h in range(4):
                        for j in range(NJ):
                            c0_ = (h * NJ + j) * 128
                            ins = e.transpose(out=pK[:, c0_:c0_ + 128], in_=kd4m[:, h, j * 128:(j + 1) * 128], identity=identb[:])
                    return ins
                S.op("tensor", trkm, reads=[b_kd4m, b_identb], writes=b_pK)
                S.op("scalar", lambda e: e.activation(
                    out=kdtm[0][0:64, :, :, :], in_=pK[0:64, 0:4 * NJ * 128].rearrange("p (h j d) -> p h j d", j=NJ, d=128), func=AF.Copy),
                    reads=b_pK, writes=b_kdtm)
                S.op("scalar", lambda e: e.activation(
                    out=kdtm[1][64:128, :, :, :], in_=pK[64:128, 0:4 * NJ * 128].rearrange("p (h j d) -> p h j d", j=NJ, d=128), func=AF.Copy),
                    reads=b_pK, writes=b_kdtm)
                def v4(bank):
                    return bank[:].rearrange("p (h e) -> p h e", e=128)
                for j in range(NJ):
                    c0 = sbf_cur[0]
                    ca = (c0 + 1) % 3
                    cb = (c0 + 2) % 3
                    sj = j % 2

                    def scf(e, j=j):
                        ins = None
                        for h in range(4):
                            ins = e.matmul(pS[:, h * 128:(h + 1) * 128], lhsT=keT[:, h, j * 128:(j + 1) * 128],
                                           rhs=qeT[:, h, j * 128:(j + 1) * 128], start=True, stop=True)
                        return ins
                    S.op("tensor", scf, reads=b_keT + b_qeT, writes=b_pS)

                    def umf(e, j=j, half=0, bank=pU):
                        ins = None
                        for h in range(4):
                            ins = e.matmul(bank[:, h * 128:(h + 1) * 128], lhsT=kdtm[half][:, h, j, :],
                                           rhs=vi[:, j, h * 128:(h + 1) * 128], start=True, stop=True)
                        return ins
                    S.op("tensor", umf, reads=b_kdtm + [b_vi[j]], writes=b_pU)
                    S.op("vector", lambda e, sj=sj: e.tensor_tensor(out=scb4[sj][:], in0=v4(pS), in1=cst[:, 1:2, :].to_broadcast([128, 4, 128]),
                                                                   op=ALU.mult), reads=b_pS + [b_cst], writes=[b_scb4[sj]])
                    for h in range(4):
                        S.op("vector", lambda e, j=j, h=h: e.scalar_tensor_tensor(
                            out=S32[:, h, :], in0=S32[:, h, :], scalar=decm[:, h, 2 * j:2 * j + 1], in1=pU[:, h * 128:(h + 1) * 128],
                            op0=ALU.mult, op1=ALU.add), reads=[b_S32[h], b_decm] + b_pU, writes=[b_S32[h]])
                    S.op("scalar", lambda e, ca=ca: e.activation(out=Sbf4[ca][:], in_=S32[:], func=AF.Copy),
                         reads=b_S32, writes=[b_Sbf4[ca]])

                    def ogf(e, j=j, sj=sj, c0=c0, ca=ca):
                        ins = None
                        for h in range(4):
                            e.matmul(pO[:, h * 128:(h + 1) * 128], lhsT=vi[:, j, h * 128:(h + 1) * 128], rhs=scb4[sj][:, h, :],
                                     start=True, stop=False)
                            e.matmul(pO[:, h * 128:h * 128 + 64], lhsT=Sbf4[c0][:, h, :], rhs=qdT[:, h, j * 128:j * 128 + 64],
                                     start=False, stop=False)
                            ins = e.matmul(pO[:, h * 128 + 64:(h + 1) * 128], lhsT=Sbf4[ca][:, h, :],
                                           rhs=qdT[:, h, j * 128 + 64:(j + 1) * 128], start=False, stop=True)
                        return ins
                    S.op("tensor", ogf, reads=[b_vi[j], b_scb4[sj], b_Sbf4[c0], b_Sbf4[ca]] + b_qdT, writes=b_pO)
                    S.op("tensor", lambda e, j=j: umf(e, j, 1, pU), reads=b_kdtm + [b_vi[j]], writes=b_pU)
                    S.op("scalar", lambda e, j=j: e.activation(out=o32[:, :, j * 128:(j + 1) * 128], in_=v4(pO), func=AF.Copy),
                         reads=b_pO, writes=b_o32)
                    for h in range(4):
                        S.op("vector", lambda e, j=j, h=h: e.scalar_tensor_tensor(
                            out=S32[:, h, :], in0=S32[:, h, :], scalar=decm[:, h, 2 * j + 1:2 * j + 2], in1=pU[:, h * 128:(h + 1) * 128],
                            op0=ALU.mult, op1=ALU.add), reads=[b_S32[h], b_decm] + b_pU, writes=[b_S32[h]])
                    S.op("scalar", lambda e, cb=cb: e.activation(out=Sbf4[cb][:], in_=S32[:], func=AF.Copy),
                         reads=b_S32, writes=[b_Sbf4[cb]])
                    sbf_cur[0] = cb
                S.op("scalar", lambda e: e.activation(out=osq4[:], in_=o32[:], func=AF.Square), reads=b_o32, writes=[b_osq4])
                backb = ((pS, b_pS), (pO, b_pO))
                for bk in range(2):
                    bkb, bbb = backb[bk]
                    S.op("tensor", lambda e, bk=bk, bkb=bkb: e.matmul(bkb[:, 0:2 * ST], lhsT=onesb[:],
                                                                      rhs=osq4[:, bk * 2:(bk + 1) * 2, :], start=True, stop=True),
                         reads=[b_onesb, b_osq4], writes=bbb)
                    S.op("scalar", lambda e, bk=bk, bkb=bkb: e.activation(out=l14m[:, bk * 2:(bk + 1) * 2, :], in_=v2(bkb), func=AF.Ln,
                                                                          scale=1.0 / 128, bias=epsb[:, 0:1]),
                         reads=bbb + [b_epsb], writes=[b_l14m])
                S.op("scalar", lambda e: e.activation(out=k4m[:], in_=l14m[:], func=AF.Exp, scale=-0.5), reads=[b_l14m], writes=[b_k4m])
                S.op("vector", lambda e: e.tensor_tensor(
                    out=d34m[:], in0=o32[:], in1=small[:, 12:16].rearrange("p (h o) -> p h o", o=1).to_broadcast([128, 4, ST]), op=ALU.mult),
                    reads=b_o32 + [b_small], writes=[b_d34m])
                S.op("vector", lambda e: e.tensor_tensor(out=d34m[:], in0=d34m[:], in1=k4m[:], op=ALU.mult),
                     reads=[b_d34m, b_k4m], writes=[b_d34m])
                S.op("gpsimd", lambda e: e.tensor_tensor(out=abT[:, 4:8, :], in0=d34m[:], in1=sgT_[:], op=ALU.mult),
                     reads=[b_d34m] + b_sgT_, writes=[x_ for c_ in range(4, 8) for x_ in b_abT[c_]])
                for j in range(NJ):
                    t = st * NJ + j
                    xs_i = t % NXS
                    hi = t % 2
                    for half in range(2):
                        bkb, bbb = backb[half]

                        def op_(e, j=j, half=half, bkb=bkb, abT=abT):
                            ins = None
                            for kc in range(8):
                                ins = e.matmul(bkb[:], lhsT=abT[:, kc, j * 128:(j + 1) * 128],
                                               rhs=wout_bf[:, kc, half * 512:(half + 1) * 512], start=(kc == 0), stop=(kc == 7))
                            return ins
                        S.op("tensor", op_, reads=[b_abT[c][j] for c in range(8)] + [b_wout], writes=bbb)
                        S.op("vector", lambda e, half=half, bkb=bkb, hi=hi, xs_i=xs_i: e.tensor_tensor(
                            out=hx[hi][:, half * 512:(half + 1) * 512], in0=bkb[:], in1=xt[xs_i][:, half * 512:(half + 1) * 512],
                            op=ALU.add), reads=bbb + [b_xt[xs_i]], writes=[b_hx[hi]])
                    S.dma("sync", lambda e, t=t, hi=hi: e.dma_start(out=hs_d[t * 128:(t + 1) * 128, :], in_=hx[hi][:]),
                          reads=[b_hx[hi]], writes=[B(f"hs_t{t}")], key=f"hs{hi}")
                    if debug:
                        S.dma("sync", lambda e, t=t, hi=hi: e.dma_start(out=dbg_h[t * 128:(t + 1) * 128, :], in_=hx[hi][:]),
                              reads=[b_hx[hi]], writes=[b_dbg], key="dbg", final=True)
                    rms_stats(t, hx[hi][:], b_hx[hi], 3, n2b, b_n2b)
                    S.op("vector", lambda e, t=t, hi=hi: e.scalar_tensor_tensor(
                        out=n2f[:], in0=hx[hi][:], scalar=stat[:, t, 5:6], in1=g2bc[:], op0=ALU.mult, op1=ALU.mult),
                        reads=[b_hx[hi], b_stat[t], b_g2bc], writes=[b_n2f])
                    S.op("scalar", lambda e: e.activation(out=n2b[:], in_=n2f[:], func=AF.Copy),
                         reads=[b_n2f], writes=[b_n2b])
                    S.dma("scalar", lambda e, t=t: e.dma_start(out=n2s_d[t * 128:(t + 1) * 128, :], in_=n2b[:]),
                          reads=[b_n2b], writes=[B(f"n2s_t{t}")], key="n2s")
                    trb = ((pU, b_pU), (pS, b_pS))
                    for half in range(2):
                        bkb, bbb = trb[half]

                        def tr2(e, half=half, bkb=bkb):
                            ins = None
                            for kk in range(4):
                                k = half * 4 + kk
                                ins = e.transpose(out=bkb[:, kk * 128:(kk + 1) * 128], in_=n2f[:, k * 128:(k + 1) * 128],
                                                  identity=cst[:, 0, :])
                            return ins
                        S.op("tensor", tr2, reads=[b_n2f, b_cst], writes=bbb)
                        S.op("vector", lambda e, half=half, bkb=bkb: e.tensor_copy(
                            out=n2T[:, half * 4:(half + 1) * 4, :], in_=bkb[:].rearrange("p (k t) -> p k t", t=128)),
                            reads=bbb, writes=[b_n2T])

                    def rt(e):
                        ins = None
                        for k in range(8):
                            ins = e.matmul(pO[:, 0:36], lhsT=n2T[:, k, :], rhs=wr[:, k, :], start=(k == 0), stop=(k == 7))
                        return ins
                    S.op("tensor", rt, reads=[b_n2T, b_wr], writes=b_pO)
                    S.op("vector", lambda e, t=t: e.tensor_tensor(out=lg[:, t, :], in0=pO[:, 0:36], in1=brbc[:], op=ALU.add),
                         reads=b_pO + [b_brbc], writes=[b_lg])
            def pre_A(pst, i2):
                nb = gcount[0] % 2
                gcount[0] += 1
                for j in range(NJ):
                    t = pst * NJ + j
                    xs_i = t % NXS
                    S.dma("sync", lambda e, t=t, xs_i=xs_i: e.dma_start(out=xt[xs_i][:], in_=xpre_d[t * 128:(t + 1) * 128, :]),
                          writes=[b_xt[xs_i]])
                    ts = NT + t % 2
                    rms_stats(ts, xt[xs_i][:], b_xt[xs_i], 0)
                    xi = t % 2
                    S.op("vector", lambda e, ts=ts, xs_i=xs_i, xi=xi: e.tensor_scalar(out=xn[xi][:], in0=xt[xs_i][:],
                                                                                     scalar1=stat[:, ts, 2:3], scalar2=None,
                                                                                     op0=ALU.mult),
                         reads=[b_xt[xs_i], b_stat[ts]], writes=[b_xn[xi]])

                    def tr(e, xi=xi):
                        ins = None
                        for k in range(8):
                            ins = e.transpose(out=pT[:, k * 128:(k + 1) * 128], in_=xn[xi][:, k * 128:(k + 1) * 128],
                                              identity=identb[:])
                        return ins
                    S.op("tensor", tr, reads=[b_xn[xi], b_identb], writes=[b_pT])
                    S.op("vector", lambda e, nb=nb, j=j: e.tensor_copy(
                        out=n1T[nb][:, :, j * 128:(j + 1) * 128], in_=pT[:].rearrange("p (k t) -> p k t", t=128)),
                        reads=[b_pT], writes=[b_n1T[nb][j]])
                emit_precast(1 + (pst % 2), b_n1T[nb][0])
                for j in range(NJ):
                    def ip(e, j=j, nb=nb):
                        ins = None
                        for k in range(8):
                            ins = e.matmul(pG[:], lhsT=n1T[nb][:, k, j * 128:(j + 1) * 128], rhs=win_bf[:, k, 2048:2560],
                                           start=(k == 0), stop=(k == 7))
                        return ins
                    S.op("tensor", ip, reads=b_win + [b_n1T[nb][j]], writes=[b_pG])
                    S.op("scalar", lambda e, j=j, i2=i2: e.activation(out=vip[i2][:, j, :], in_=pG[:], func=AF.Copy),
                         reads=[b_pG], writes=[b_vip[i2]])
                for bk in range(2):
                    def fp(e, bk=bk, nb=nb):
                        ins = None
                        for hh in range(2):
                            h = bk * 2 + hh
                            for k in range(8):
                                ins = e.matmul(pP[bk][:, hh * ST:(hh + 1) * ST], lhsT=win_bf[:, k, 1536 + h * 128:1536 + (h + 1) * 128],
                                               rhs=n1T[nb][:, k, :], start=(k == 0), stop=(k == 7))
                        return ins
                    S.op("tensor", fp, reads=b_win + b_n1T[nb], writes=[b_pP[bk]])
                    S.op("scalar", lambda e, bk=bk, i2=i2: e.activation(
                        out=sg4[i2][:, bk * 2:(bk + 1) * 2, :], in_=pP[bk][:, 0:2 * ST].rearrange("p (h t) -> p h t", t=ST), func=AF.Exp,
                        scale=-1.0), reads=[b_pP[bk]], writes=[b_sg4[i2]])
                S.op("scalar", lambda e, i2=i2: e.activation(out=sg4[i2][:], in_=sg4[i2][:], func=AF.Ln, bias=oneb[:, 0:1]),
                     reads=[b_sg4[i2]], writes=[b_sg4[i2]])
                S.op("scalar", lambda e, i2=i2: e.activation(out=sg4[i2][:], in_=sg4[i2][:], func=AF.Exp, scale=-1.0),
                     reads=[b_sg4[i2]], writes=[b_sg4[i2]])

            banks3 = ((pS, b_pS), (pO, b_pO), (pU, b_pU))
            bank_ctr = [0]

            def pre_B(pst, i2, last):
                for h in range(4):
                    S.op("scalar", lambda e, h=h, i2=i2: e.activation(out=l14[:, h, :], in_=sg4[i2][:, h, :], func=AF.Ln,
                                                                       scale=lbt[:, 4 + h:5 + h], bias=lbt[:, h:h + 1]),
                         reads=[b_sg4[i2], b_lbt], writes=[b_l14])
                for h in range(4):
                    S.op("vector", lambda e, i2=i2, h=h: e.tensor_scalar(
                        out=k4[:, h, :], in0=sg4[i2][:, h, :], scalar1=lbt[:, 8 + h:9 + h], scalar2=lbt[:, 4 + h:5 + h],
                        op0=ALU.mult, op1=ALU.add), reads=[b_sg4[i2], b_lbt], writes=[b_k4])
                S.op("vector", lambda e: e.tensor_tensor_scan(out=bT4[:].rearrange("p h t -> p (h t)"), data0=rmask[:],
                                                              data1=l14[:].rearrange("p h t -> p (h t)"), initial=0.0,
                                                              op0=ALU.mult, op1=ALU.add),
                     reads=[b_l14, b_rmask], writes=[b_bT4])
                bv4 = bT4[:].rearrange("p h (c t) -> p (h c) t", t=64)
                S.op("vector", lambda e: e.tensor_tensor(
                    out=d34[:].rearrange("p h (c t) -> p (h c) t", t=64), in0=bv4,
                    in1=bv4[:, :, 63:64].to_broadcast([128, 4 * NCH, 64]), op=ALU.subtract),
                    reads=[b_bT4], writes=[b_d34])
                S.op("vector", lambda e: e.tensor_copy(out=blp[:].rearrange("p h c -> p (h c)"), in_=bv4[:, :, 63]),
                     reads=[b_bT4], writes=[b_blp])
                S.op("scalar", lambda e: e.activation(out=d34[:], in_=d34[:], func=AF.Exp, scale=-1.0), reads=[b_d34], writes=[b_d34])
                S.op("scalar", lambda e: e.activation(out=decp[:], in_=blp[:], func=AF.Exp), reads=[b_blp], writes=[b_decp])
                S.op("gpsimd", lambda e: e.tensor_tensor(out=kd4[:], in0=k4[:], in1=d34[:], op=ALU.mult),
                     reads=[b_k4, b_d34], writes=[b_kd4])

                def trk(e):
                    ins = None
                    for h in range(4):
                        for j in range(NJ):
                            c0_ = (h * NJ + j) * 128
                            ins = e.transpose(out=pK[:, c0_:c0_ + 128], in_=kd4[:, h, j * 128:(j + 1) * 128], identity=identb[:])
                    return ins
                S.op("tensor", trk, reads=[b_kd4, b_identb], writes=b_pK)
                S.op("scalar", lambda e: e.activation(
                    out=kdtm[0][0:64, :, :, :], in_=pK[0:64, 0:4 * NJ * 128].rearrange("p (h j d) -> p h j d", j=NJ, d=128), func=AF.Copy),
                    reads=b_pK, writes=b_kdtm)
                S.op("scalar", lambda e: e.activation(
                    out=kdtm[1][64:128, :, :, :], in_=pK[64:128, 0:4 * NJ * 128].rearrange("p (h j d) -> p h j d", j=NJ, d=128), func=AF.Copy),
                    reads=b_pK, writes=b_kdtm)
                for j in range(NJ):
                    for half in range(2):
                        c = j * 2 + half
                        bank, b_bank = banks3[bank_ctr[0] % 3]
                        bank_ctr[0] += 1

                        def um(e, j=j, half=half, bank=bank, i2=i2):
                            ins = None
                            for h in range(4):
                                ins = e.matmul(bank[:, h * 128:(h + 1) * 128], lhsT=kdtm[half][:, h, j, :],
                                               rhs=vip[i2][:, j, h * 128:(h + 1) * 128], start=True, stop=True)
                            return ins
                        S.op("tensor", um, reads=b_kdtm + [b_vip[i2]], writes=b_bank)
                        for h in range(4):
                            S.op("vector", lambda e, c=c, h=h, bank=bank: e.scalar_tensor_tensor(
                                out=S32[:, h, :], in0=S32[:, h, :], scalar=decp[:, h, c:c + 1], in1=bank[:, h * 128:(h + 1) * 128],
                                op0=ALU.mult, op1=ALU.add), reads=[b_S32[h], b_decp] + b_bank, writes=[b_S32[h]])
                if last:
                    S.op("scalar", lambda e: e.activation(out=Sbf4[0][:], in_=S32[:], func=AF.Copy),
                         reads=b_S32, writes=[b_Sbf4[0]])

            kpre = int(os.environ.get("KPRE", NPRE))
            plist = list(range(NPRE - kpre, NPRE))
            for n, pst in enumerate(plist):
                pre_A(pst, n % 2)
                if n > 0:
                    pre_B(plist[n - 1], (n - 1) % 2, False)
            if plist:
                pre_B(plist[-1], (len(plist) - 1) % 2, True)
            with nc.Block() as block:
                S.emit(block)
            s1a.close()
            Buf.reset_all()
            o32 = sb(s1, "o32", [128, 4, ST], F32)
            qs4 = sb(s1, "qs4", [128, 4, ST], F32)
            sg4m = sb(s1, "sg4m", [128, 4, ST], F32)
            l14m = sb(s1, "l14m", [128, 4, ST], F32)
            k4m = sb(s1, "k4m", [128, 4, ST], F32)
            bT4m = sb(s1, "bT4m", [128, 4, ST], F32)
            d34m = sb(s1, "d34m", [128, 4, ST], F32)
            E12m = sb(s1, "E12m", [128, 4, ST], F32)
            kd4m = sb(s1, "kd4m", [128, 4, ST], BF16)
            osq4 = sb(s1, "osq4", [128, 4, ST], BF16)
            blm = sb(s1, "blm", [128, 4, NCH], F32)
            decm = sb(s1, "decm", [128, 4, NCH], F32)
            scb4 = [sb(s1, f"scb4{i}", [128, 4, 128], BF16) for i in range(2)]
            b_qs4 = B("qs4"); b_sg4m = B("sg4m"); b_l14m = B("l14m"); b_k4m = B("k4m"); b_bT4m = B("bT4m"); b_d34m = B("d34m")
            b_E12m = B("E12m"); b_kd4m = B("kd4m"); b_osq4 = B("osq4"); b_blm = B("blm"); b_decm = B("decm")
            b_scb4 = [B("scb40"), B("scb41")]
            abT2 = [sb(s1, f"abT{i}", [128, 8, ST], BF16) for i in range(2)]
            b_abT2 = [[[B(f"abT{i}_{c}_{j}") for j in range(NJ)] for c in range(8)] for i in range(2)]
            sgT2 = [sgT, sb(s1, "sgTb", [128, 4, ST], BF16)]
            b_sgT2 = [b_sgT, [B(f"sgTb{g}") for g in range(4)]]
            hx = [sb(s1, f"hx{i}", [128, D], F32) for i in range(2)]
            n2f = sb(s1, "n2f", [128, D], F32)
            n2T = sb(s1, "n2T", [128, 8, 128], F32)
            S = Sched(nc, s1, "m")
            gcount[0] = 0
            for st in range(NST):
                super_tile(st, False)
            emit_precast(len(precast), b_lg)
            with nc.Block() as block:
                S.emit(block)

        with ExitStack() as s2:
            if phases < 2:
                return nc
            S = Sched(nc, s2, "b")
            B = Buf
            gfbc = sb(s2, "gfbc", [128, D], F32)
            blkthr = sb(s2, "blkthr", [128, NB], F32)
            onesb2 = sb(s2, "onesb2", [128, 128], BF16)
            trib = sb(s2, "trib", [128, 128], BF16)
            identb2 = sb(s2, "identb2", [128, 128], BF16)
            R = sb(s2, "R", [128, 24, NT], F32)
            ohg = sb(s2, "ohg", [128, NT, 4], F32)
            gex = sb(s2, "gex", [128, NT, 4], F32)
            pen = sb(s2, "pen", [128, NT, 32], F32)
            elm = sb(s2, "elm", [128, NT, 32], F32)
            elm2 = sb(s2, "elm2", [128, NT, 32], F32)
            oh1 = sb(s2, "oh1", [128, NT, 32], F32)
            oh2 = sb(s2, "oh2", [128, NT, 32], F32)
            ohb = sb(s2, "ohb", [128, NT, 32], BF16)
            cum = sb(s2, "cum", [128, NT, 32], F32)
            prod = sb(s2, "prod", [128, NT, 32], F32)
            cnt = sb(s2, "cnt", [128, 6, 32], F32)
            ebt = sb(s2, "ebt", [128, 2, NB], F32)
            widx = [sb(s2, f"widx{b}", [128, 1], I32) for b in range(NB)]
            dst = [[sb(s2, f"dst{k}_{t}", [128, 1], I32) for t in range(NT)] for k in range(2)]
            n2r = [sb(s2, f"n2r{i}", [128, D], BF16) for i in range(2)]
            NWB = 3
            wbf = [[sb(s2, f"wbf{i}_{p}", [128, 4096], BF16) for p in range(3)] for i in range(NWB)]
            xb = [sb(s2, f"xb{i}", [128, 2, D], BF16) for i in range(2)]
            xT = [sb(s2, f"xT{i}", [128, 8, BLK], BF16) for i in range(2)]
            sg = [sb(s2, f"sg{i}", [128, BLK], F32) for i in range(2)]
            hT = [sb(s2, f"hT{i}", [128, 4, BLK], BF16) for i in range(2)]
            yo = [sb(s2, f"yo{i}", [128, D], F32) for i in range(2)]
            hq = [sb(s2, f"hq{i}", [128, D], F32) for i in range(3)]
            y1 = [sb(s2, f"y1{i}", [128, D], F32) for i in range(3)]
            y2 = [sb(s2, f"y2{i}", [128, D], F32) for i in range(3)]
            sq2 = sb(s2, "sq2", [128, D], BF16)
            fst = sb(s2, "fst", [128, NT, 4], F32)
            ot = [sb(s2, f"ot{i}", [128, D], F32) for i in range(3)]
            pC = ps(s2, "pC", [128, 512], F32)
            pTot = ps(s2, "pTot", [128, 512], F32)
            pX = [ps(s2, f"pX{i}", [128, 1024], BF16) for i in range(2)]
            pGt = [ps(s2, f"pGt{i}", [128, 512], F32) for i in range(2)]
            pY = [ps(s2, f"pY{i}", [128, 512], F32) for i in range(2)]

            b_cst = B("cst"); b_small = B("small"); b_lg = B("lg")
            b_gfbc = B("gfbc"); b_blkthr = B("blkthr"); b_onesb2 = B("onesb2"); b_trib = B("trib"); b_identb2 = B("identb2")
            b_R = B("R"); b_ohg = B("ohg"); b_gex = B("gex"); b_pen = B("pen"); b_elm = B("elm"); b_elm2 = B("elm2")
            b_oh1 = B("oh1"); b_oh2 = B("oh2"); b_ohb = B("ohb"); b_cum = B("cum"); b_prod = B("prod"); b_cnt = B("cnt")
            b_ebt = B("ebt")
            b_widx = [B(f"widx{b}") for b in range(NB)]
            b_dst = [[B(f"dst{k}_{t}") for t in range(NT)] for k in range(2)]
            b_n2r = [B(f"n2r{i}") for i in range(2)]
            b_wbf = [[B(f"wbf{i}_{p}") for p in range(3)] for i in range(NWB)]
            b_xb = [B(f"xb{i}") for i in range(2)]
            b_xT = [B(f"xT{i}") for i in range(2)]
            b_sg = [B(f"sg{i}") for i in range(2)]
            b_hT = [B(f"hT{i}") for i in range(2)]
            b_yo = [B(f"yo{i}") for i in range(2)]
            b_hq = [B(f"hq{i}") for i in range(3)]
            b_y1 = [B(f"y1{i}") for i in range(3)]
            b_y2 = [B(f"y2{i}") for i in range(3)]
            b_sq2 = B("sq2")
            b_fst = [B(f"fst{t}") for t in range(NT)]
            b_ot = [B(f"ot{i}") for i in range(3)]
            b_pC = B("pC"); b_pTot = B("pTot")
            b_pX = [B("pX0"), B("pX1")]
            b_pGt = [B("pGt0"), B("pGt1")]
            b_pY = [B("pY0"), B("pY1")]
            b_xsl = [B(f"xs_{i}") for i in range(2 * NT)]
            b_ysl = [B(f"ys_{i}") for i in range(2 * NB)]

            S.dma("sync", lambda e: e.dma_start(out=gfbc[:], in_=gfbc_d[:, :]), writes=[b_gfbc])
            S.dma("sync", lambda e: e.dma_start(out=blkthr[:], in_=blkthr_d[:, :]), writes=[b_blkthr])
            S.op("vector", lambda e: e.tensor_copy(out=identb2[:], in_=cst[:, 0, :]), reads=[b_cst], writes=[b_identb2])
            S.op("vector", lambda e: e.tensor_copy(out=trib[:], in_=cst[:, 3, :]), reads=[b_cst], writes=[b_trib])
            S.op("vector", lambda e: e.tensor_copy(out=onesb2[:], in_=cst[:, 4, :]), reads=[b_cst], writes=[b_onesb2])

            gl = lg[:, :, 0:4]
            el = lg[:, :, 4:36]
            V = S.op
            V("vector", lambda e: e.tensor_reduce(out=R[:, 0, :], in_=gl, axis=AX.X, op=ALU.max), reads=[b_lg], writes=[b_R])
            V("vector", lambda e: e.tensor_tensor(out=ohg[:], in0=gl, in1=R[:, 0, :].rearrange("p (t o) -> p t o", o=1).to_broadcast([128, NT, 4]),
                                                  op=ALU.is_equal), reads=[b_lg, b_R], writes=[b_ohg])
            V("vector", lambda e: e.tensor_tensor(out=gex[:], in0=gl, in1=R[:, 0, :].rearrange("p (t o) -> p t o", o=1).to_broadcast([128, NT, 4]),
                                                  op=ALU.subtract), reads=[b_lg, b_R], writes=[b_gex])
            V("scalar", lambda e: e.activation(out=gex[:], in_=gex[:], func=AF.Exp), reads=[b_gex], writes=[b_gex])
            V("vector", lambda e: e.tensor_reduce(out=R[:, 1, :], in_=gex[:], axis=AX.X, op=ALU.add), reads=[b_gex], writes=[b_R])
            V("vector", lambda e: e.reciprocal(out=R[:, 2, :], in_=R[:, 1, :]), reads=[b_R], writes=[b_R])
            V("vector", lambda e: e.tensor_scalar(
                out=pen[:].rearrange("p t (g j) -> p t g j", j=8),
                in0=ohg[:].rearrange("p t (g o) -> p t g o", o=1).to_broadcast([128, NT, 4, 8]),
                scalar1=-1.0, scalar2=BIG, op0=ALU.add, op1=ALU.mult), reads=[b_ohg], writes=[b_pen])
            V("vector", lambda e: e.tensor_tensor(out=elm[:], in0=el, in1=pen[:], op=ALU.add), reads=[b_lg, b_pen], writes=[b_elm])
            V("vector", lambda e: e.tensor_reduce(out=R[:, 3, :], in_=elm[:], axis=AX.X, op=ALU.max), reads=[b_elm], writes=[b_R])
            V("vector", lambda e: e.tensor_tensor(out=oh1[:], in0=elm[:], in1=R[:, 3, :].rearrange("p (t o) -> p t o", o=1).to_broadcast([128, NT, 32]),
                                                  op=ALU.is_equal), reads=[b_elm, b_R], writes=[b_oh1])
            V("vector", lambda e: e.scalar_tensor_tensor(out=elm2[:], in0=oh1[:], scalar=-BIG, in1=elm[:], op0=ALU.mult, op1=ALU.add),
              reads=[b_oh1, b_elm], writes=[b_elm2])
            V("vector", lambda e: e.tensor_reduce(out=R[:, 4, :], in_=elm2[:], axis=AX.X, op=ALU.max), reads=[b_elm2], writes=[b_R])
            V("vector", lambda e: e.tensor_tensor(out=oh2[:], in0=elm2[:], in1=R[:, 4, :].rearrange("p (t o) -> p t o", o=1).to_broadcast([128, NT, 32]),
                                                  op=ALU.is_equal), reads=[b_elm2, b_R], writes=[b_oh2])
            V("vector", lambda e: e.tensor_tensor(out=R[:, 5, :], in0=R[:, 4, :], in1=R[:, 3, :], op=ALU.subtract), reads=[b_R], writes=[b_R])
            V("scalar", lambda e: e.activation(out=R[:, 5, :], in_=R[:, 5, :], func=AF.Exp), reads=[b_R], writes=[b_R])
            V("vector", lambda e: e.tensor_scalar(out=R[:, 5, :], in0=R[:, 5, :], scalar1=1.0, scalar2=None, op0=ALU.add), reads=[b_R], writes=[b_R])
            V("vector", lambda e: e.reciprocal(out=R[:, 6, :], in_=R[:, 5, :]), reads=[b_R], writes=[b_R])
            V("vector", lambda e: e.tensor_scalar(out=R[:, 7, :], in0=R[:, 6, :], scalar1=-1.0, scalar2=1.0, op0=ALU.mult, op1=ALU.add),
              reads=[b_R], writes=[b_R])
            V("vector", lambda e: e.tensor_tensor(out=R[:, 8, :], in0=R[:, 6, :], in1=R[:, 2, :], op=ALU.mult), reads=[b_R], writes=[b_R])
            V("vector", lambda e: e.tensor_tensor(out=R[:, 9, :], in0=R[:, 7, :], in1=R[:, 2, :], op=ALU.mult), reads=[b_R], writes=[b_R])
            V("vector", lambda e: e.tensor_tensor(out=ohb[:], in0=oh1[:], in1=oh2[:], op=ALU.add), reads=[b_oh1, b_oh2], writes=[b_ohb])

            def cumf(e):
                ins = None
                for i in range(NT):
                    for i2 in range(i):
                        e.matmul(pC[:, i * 32:(i + 1) * 32], lhsT=onesb2[:], rhs=ohb[:, i2, :], start=(i2 == 0), stop=False)
                    ins = e.matmul(pC[:, i * 32:(i + 1) * 32], lhsT=trib[:], rhs=ohb[:, i, :], start=(i == 0), stop=True)
                return ins
            V("tensor", cumf, reads=[b_ohb, b_onesb2, b_trib], writes=[b_pC])

            def totf(e):
                ins = None
                for i in range(NT):
                    ins = e.matmul(pTot[:, 0:32], lhsT=onesb2[:], rhs=ohb[:, i, :], start=(i == 0), stop=(i == NT - 1))
                return ins
            V("tensor", totf, reads=[b_ohb, b_onesb2], writes=[b_pTot])
            V("vector", lambda e: e.tensor_copy(out=cum[:], in_=pC[:].rearrange("p (t x) -> p t x", x=32)), reads=[b_pC], writes=[b_cum])
            V("vector", lambda e: e.tensor_copy(out=cnt[:, 0, :], in_=pTot[:, 0:32]), reads=[b_pTot], writes=[b_cnt])
            V("vector", lambda e: e.memset(cnt[:, 1, :], 0.0), reads=[b_cnt], writes=[b_cnt])
            for m in range(TOK // BLK):
                V("vector", lambda e, m=m: e.scalar_tensor_tensor(out=cnt[:, 1, :], in0=cnt[:, 0, :], scalar=float(m * BLK) + 0.5,
                                                                  in1=cnt[:, 1, :], op0=ALU.is_gt, op1=ALU.add),
                  reads=[b_cnt], writes=[b_cnt])
            V("vector", lambda e: e.tensor_scalar(out=cnt[:, 2, :], in0=cnt[:, 1, :], scalar1=float(BLK), scalar2=None, op0=ALU.mult),
              reads=[b_cnt], writes=[b_cnt])
            V("vector", lambda e: e.memset(cnt[:, 5, :], 1.0), reads=[b_cnt], writes=[b_cnt])
            V("vector", lambda e: e.tensor_tensor_scan(out=cnt[:, 3, :], data0=cnt[:, 5, :], data1=cnt[:, 2, :], initial=0.0,
                                                       op0=ALU.mult, op1=ALU.add), reads=[b_cnt], writes=[b_cnt])
            V("vector", lambda e: e.tensor_tensor(out=cnt[:, 4, :], in0=cnt[:, 3, :], in1=cnt[:, 2, :], op=ALU.subtract),
              reads=[b_cnt], writes=[b_cnt])
            V("vector", lambda e: e.tensor_tensor(out=cum[:], in0=cum[:], in1=cnt[:, 4:5, :].to_broadcast([128, NT, 32]), op=ALU.add),
              reads=[b_cum, b_cnt], writes=[b_cum])
            for k, ohk in ((0, oh1), (1, oh2)):
                V("vector", lambda e, ohk=ohk: e.tensor_tensor(out=prod[:], in0=cum[:], in1=ohk[:], op=ALU.mult),
                  reads=[b_cum, b_oh1, b_oh2], writes=[b_prod])
                V("vector", lambda e, k=k: e.tensor_reduce(out=R[:, 10 + k, :], in_=prod[:], axis=AX.X, op=ALU.add),
                  reads=[b_prod], writes=[b_R])
                for t in range(NT):
                    V("vector", lambda e, k=k, t=t: e.tensor_copy(out=dst[k][t][:], in_=R[:, 10 + k, t:t + 1]),
                      reads=[b_R], writes=[b_dst[k][t]])
            V("vector", lambda e: e.memset(ebt[:, 0, :], 0.0), writes=[b_ebt])
            for ex in range(32):
                V("vector", lambda e, ex=ex: e.scalar_tensor_tensor(out=ebt[:, 0, :], in0=blkthr[:], scalar=cnt[:, 3, ex:ex + 1],
                                                                    in1=ebt[:, 0, :], op0=ALU.is_ge, op1=ALU.add),
                  reads=[b_blkthr, b_cnt, b_ebt], writes=[b_ebt])
            V("vector", lambda e: e.tensor_scalar(out=ebt[:, 0, :], in0=ebt[:, 0, :], scalar1=128.0, scalar2=None, op0=ALU.mult),
              reads=[b_ebt], writes=[b_ebt])
            V("vector", lambda e: e.tensor_scalar(out=ebt[:, 1, :], in0=ebt[:, 0, :], scalar1=small[:, 24:25], scalar2=None, op0=ALU.add),
              reads=[b_ebt, b_small], writes=[b_ebt])
            for b in range(NB):
                V("vector", lambda e, b=b: e.tensor_copy(out=widx[b][:], in_=ebt[:, 1, b:b + 1]), reads=[b_ebt], writes=[b_widx[b]])
            if debug:
                S.dma("sync", lambda e: e.dma_start(out=dbg_lg[:, :, :], in_=lg[:]), reads=[b_lg], writes=[B("dl")], key="dl", final=True)
                S.dma("sync", lambda e: e.dma_start(out=dbg_r[:, :, :], in_=R[:, 4:12, :]), reads=[b_R] + b_dst[1], writes=[B("dr")], key="dr", final=True)

            for t in range(NT):
                i = t % 2
                S.dma("sync", lambda e, t=t, i=i: e.dma_start(out=n2r[i][:], in_=n2s_d[t * 128:(t + 1) * 128, :]), writes=[b_n2r[i]])
                for k in range(2):
                    S.dma("gpsimd", lambda e, t=t, i=i, k=k: e.indirect_dma_start(
                        out=xs_d[:, :], out_offset=bass.IndirectOffsetOnAxis(ap=dst[k][t][:, :], axis=0),
                        in_=n2r[i][:], in_offset=None, bounds_check=_bc(e, NSLOT - 1), oob_is_err=False),
                        reads=[b_n2r[i], b_dst[k][t]], writes=[b_xsl[2 * t + k]], key=f"xs{i}")

            for b in range(NB):
                i = b % 2
                wi = b % NWB
                for p, wsrc in enumerate((wgb_d, wub_d, wdb_d)):
                    S.dma("gpsimd", lambda e, b=b, p=p, wsrc=wsrc, wi=wi: e.indirect_dma_start(
                        out=wbf[wi][p][:], out_offset=None, in_=wsrc[:, :],
                        in_offset=bass.IndirectOffsetOnAxis(ap=widx[b][:, :], axis=0), bounds_check=_bc(e, 32 * 128 - 1), oob_is_err=False),
                        reads=[b_widx[b]], writes=[b_wbf[wi][p]])
                S.dma("sync", lambda e, b=b, i=i: e.dma_start(
                    out=xb[i][:], in_=xs_d[b * BLK:(b + 1) * BLK, :].rearrange("(s p) d -> p s d", p=128)),
                    reads=b_xsl, writes=[b_xb[i]])
                for s in range(2):
                    def trx(e, i=i, s=s):
                        ins = None
                        for k in range(8):
                            ins = e.transpose(out=pX[s][:, k * 128:(k + 1) * 128], in_=xb[i][:, s, k * 128:(k + 1) * 128],
                                              identity=identb2[:])
                        return ins
                    S.op("tensor", trx, reads=[b_xb[i], b_identb2], writes=[b_pX[s]])
                    S.op("vector" if s == 0 else "gpsimd" if False else "vector", lambda e, i=i, s=s: e.tensor_copy(
                        out=xT[i][:, :, s * 128:(s + 1) * 128], in_=pX[s][:].rearrange("p (k t) -> p k t", t=128)),
                        reads=[b_pX[s]], writes=[b_xT[i]])
                wgv = wbf[wi][0][:].rearrange("p (k f) -> p k f", f=512)
                wuv = wbf[wi][1][:].rearrange("p (k f) -> p k f", f=512)
                wdv = wbf[wi][2][:].rearrange("p (c d) -> p c d", d=1024)
                for fc in range(4):
                    def gmm(e, wv, pi, fc=fc, i=i):
                        ins = None
                        for k in range(8):
                            ins = e.matmul(pGt[pi][:, 0:BLK], lhsT=wv[:, k, fc * 128:(fc + 1) * 128], rhs=xT[i][:, k, :],
                                           start=(k == 0), stop=(k == 7))
                        return ins
                    S.op("tensor", lambda e, fc=fc, wgv=wgv, i=i, gmm=gmm: gmm(e, wgv, 0, fc, i), reads=[b_wbf[wi][0], b_xT[i]], writes=[b_pGt[0]])
                    S.op("tensor", lambda e, fc=fc, wuv=wuv, i=i, gmm=gmm: gmm(e, wuv, 1, fc, i), reads=[b_wbf[wi][1], b_xT[i]], writes=[b_pGt[1]])
                    si = fc % 2
                    S.op("scalar", lambda e, si=si: e.activation(out=sg[si][:], in_=pGt[0][:, 0:BLK], func=AF.Silu),
                         reads=[b_pGt[0]], writes=[b_sg[si]])
                    S.op("vector", lambda e, si=si, fc=fc, i=i: e.tensor_tensor(out=hT[i][:, fc, :], in0=pGt[1][:, 0:BLK], in1=sg[si][:], op=ALU.mult),
                         reads=[b_pGt[1], b_sg[si]], writes=[b_hT[i]])
                for s in range(2):
                    yi = s
                    for half in range(2):
                        def dmm(e, s=s, half=half, i=i, wdv=wdv):
                            ins = None
                            for fc in range(4):
                                ins = e.matmul(pY[half][:], lhsT=hT[i][:, fc, s * 128:(s + 1) * 128],
                                               rhs=wdv[:, fc, half * 512:(half + 1) * 512], start=(fc == 0), stop=(fc == 3))
                            return ins
                        S.op("tensor", dmm, reads=[b_hT[i], b_wbf[wi][2]], writes=[b_pY[half]])
                        if half == 0:
                            S.op("scalar", lambda e, yi=yi: e.activation(out=yo[yi][:, 0:512], in_=pY[0][:], func=AF.Copy),
                                 reads=[b_pY[0]], writes=[b_yo[yi]])
                        else:
                            S.op("vector", lambda e, yi=yi: e.tensor_copy(out=yo[yi][:, 512:1024], in_=pY[1][:]),
                                 reads=[b_pY[1]], writes=[b_yo[yi]])
                    S.dma("sync", lambda e, b=b, s=s, yi=yi: e.dma_start(out=ys_d[b * BLK + s * 128: b * BLK + (s + 1) * 128, :], in_=yo[yi][:]),
                          reads=[b_yo[yi]], writes=[b_ysl[2 * b + s]], key=f"ys{yi}")

            for t in range(NT):
                i = t % 3
                S.dma("sync", lambda e, t=t, i=i: e.dma_start(out=hq[i][:], in_=hs_d[t * 128:(t + 1) * 128, :]), writes=[b_hq[i]])
                for k, (yk, b_yk) in enumerate(((y1, b_y1), (y2, b_y2))):
                    S.dma("gpsimd", lambda e, t=t, i=i, k=k, yk=yk: e.indirect_dma_start(
                        out=yk[i][:], out_offset=None, in_=ys_d[:, :],
                        in_offset=bass.IndirectOffsetOnAxis(ap=dst[k][t][:, :], axis=0), bounds_check=_bc(e, NSLOT - 1), oob_is_err=False),
                        reads=b_ysl + [b_dst[k][t]], writes=[b_yk[i]])
                S.op("vector", lambda e, t=t, i=i: e.scalar_tensor_tensor(out=hq[i][:], in0=y1[i][:], scalar=R[:, 8, t:t + 1], in1=hq[i][:],
                                                                         op0=ALU.mult, op1=ALU.add),
                     reads=[b_y1[i], b_R, b_hq[i]], writes=[b_hq[i]])
                S.op("vector", lambda e, t=t, i=i: e.scalar_tensor_tensor(out=hq[i][:], in0=y2[i][:], scalar=R[:, 9, t:t + 1], in1=hq[i][:],
                                                                         op0=ALU.mult, op1=ALU.add),
                     reads=[b_y2[i], b_R, b_hq[i]], writes=[b_hq[i]])
                S.op("scalar", lambda e, t=t, i=i: e.activation(out=sq2[:], in_=hq[i][:], func=AF.Square, accum_out=fst[:, t, 0:1]),
                     reads=[b_hq[i]], writes=[b_sq2, b_fst[t]])
                S.op("scalar", lambda e, t=t: e.activation(out=fst[:, t, 1:2], in_=fst[:, t, 0:1], func=AF.Ln, scale=1.0 / D, bias=epsb[:, 0:1]),
                     reads=[b_fst[t]], writes=[b_fst[t]])
                S.op("scalar", lambda e, t=t: e.activation(out=fst[:, t, 2:3], in_=fst[:, t, 1:2], func=AF.Exp, scale=-0.5),
                     reads=[b_fst[t]], writes=[b_fst[t]])
                S.op("vector", lambda e, t=t, i=i: e.scalar_tensor_tensor(out=ot[i][:], in0=hq[i][:], scalar=fst[:, t, 2:3], in1=gfbc[:],
                                                                         op0=ALU.mult, op1=ALU.mult),
                     reads=[b_hq[i], b_fst[t], b_gfbc], writes=[b_ot[i]])
                S.dma("sync", lambda e, t=t, i=i: e.dma_start(out=out_d[t * 128:(t + 1) * 128, :], in_=ot[i][:]),
                      reads=[b_ot[i]], writes=[B(f"out_t{t}")], key=f"out{i}", final=True)
            with nc.Block() as block:
                S.emit(block)
    return nc


def _host_inputs(inp):
    f = np.float32
    w_in = np.asarray(inp["w_in"][0], f)
    win = np.ascontiguousarray(w_in.reshape(8, 128, 3072).transpose(1, 0, 2))
    wout = np.ascontiguousarray(np.asarray(inp["w_out"][0], f).reshape(8, 128, 1024).transpose(1, 0, 2))
    wsT = np.ascontiguousarray(np.asarray(inp["gmlp_w_s"][0], f).transpose(2, 0, 1))
    bsbc = np.ascontiguousarray(np.broadcast_to(np.asarray(inp["gmlp_b_s"][0], f)[None], (128, 4, 128)))
    small = np.zeros((128, 32), f)
    small[:, 0:4] = np.asarray(inp["gmlp_v_gain"][0], f).reshape(4, 128).T
    lbr = np.asarray(inp["hgrn_lower_bounds"], f)
    small[:, 4:12] = lbr.reshape(2, 4, 128).transpose(2, 1, 0).reshape(128, 8)
    small[:, 12:16] = np.asarray(inp["hgrn_out_gain"][0], f).reshape(4, 128).T
    small[:, 16:24] = np.asarray(inp["norm1_gain"][0], f).reshape(8, 128).T
    small[:, 24] = np.arange(128, dtype=f)
    g2bc = np.ascontiguousarray(np.broadcast_to(np.asarray(inp["norm2_gain"][0], f)[None], (128, D)))
    gfbc = np.ascontiguousarray(np.broadcast_to(np.asarray(inp["final_gain"], f)[None], (128, D)))
    wrf = np.concatenate([np.asarray(inp["w_group_router"][0], f), np.asarray(inp["w_expert_router"][0], f)], axis=1)
    wr = np.ascontiguousarray(wrf.reshape(8, 128, 36).transpose(1, 0, 2))
    br = np.concatenate([np.asarray(inp["b_group_router"][0], f), np.asarray(inp["b_expert_router"][0], f)])
    brbc = np.ascontiguousarray(np.broadcast_to(br[None], (128, 36)))
    idx = np.arange(128)
    consts = np.zeros((128, 5, 128), f)
    consts[:, 0, :] = np.eye(128, dtype=f)
    consts[:, 1, :] = ((idx[:, None] // 64 == idx[None, :] // 64) & (idx[:, None] <= idx[None, :])).astype(f)
    consts[:, 2, :] = (idx[:, None] // 64 <= idx[None, :] // 64).astype(f)
    consts[:, 3, :] = (idx[:, None] < idx[None, :]).astype(f)
    consts[:, 4, :] = 1.0
    rmask = np.ones((128, 1024), f)
    rmask[:, 0::64] = 0.0
    blkthr = np.ascontiguousarray(np.broadcast_to((np.arange(NB, dtype=f) * BLK)[None], (128, NB)))
    wg = np.ascontiguousarray(np.asarray(inp["w_gate"][0], f).reshape(32, 8, 128, 512).transpose(0, 2, 1, 3)).reshape(32 * 128, 4096)
    wu = np.ascontiguousarray(np.asarray(inp["w_up"][0], f).reshape(32, 8, 128, 512).transpose(0, 2, 1, 3)).reshape(32 * 128, 4096)
    wd = np.ascontiguousarray(np.asarray(inp["w_down"][0], f).reshape(32, 4, 128, 1024).transpose(0, 2, 1, 3)).reshape(32 * 128, 4096)
    return dict(win=win, wout=wout, wsT=wsT, bsbc=bsbc, small=small, g2bc=g2bc, gfbc=gfbc, wr=wr, brbc=brbc,
                consts=consts, rmask=rmask, blkthr=blkthr, wg=wg, wu=wu, wd=wd)


def _prefix(x, c):
    p = c % 4
    pre = np.zeros((3 * TOK, D), np.float32)
    if p > 0:
        prev = x[c - p:c].reshape(p * TOK, D)
        pre[(3 - p) * TOK:] = prev
    return pre


_NC_CACHE = {}


def kernel(**inputs):
    x = np.asarray(inputs["x"], np.float32).reshape(NCORES, TOK, D)
    shared = _host_inputs(inputs)
    if "nc" not in _NC_CACHE:
        _NC_CACHE["nc"] = build(phases=int(os.environ.get("KPH", "2")))
    nc = _NC_CACHE["nc"]
    in_maps = []
    for c in range(NCORES):
        m = dict(shared)
        if int(os.environ.get("KPH", "2")) < 2:
            for kk in ("wg", "wu", "wd"):
                m.pop(kk, None)
        m["x"] = np.ascontiguousarray(x[c])
        m["xpre"] = _prefix(x, c)
        in_maps.append(m)
    res = run_bass_kernel_spmd(nc, in_maps, core_ids=list(range(NCORES)))
    out = np.stack([np.asarray(r["out"], np.float32) for r in res.results], axis=0)
    return out.reshape(2, 8192, D)
```

```python
from contextlib import ExitStack
import os
import numpy as np
import concourse.bass as bass
import concourse.mybir as mybir
from concourse.bass_utils import run_bass_kernel_spmd

F32 = mybir.dt.float32
BF16 = mybir.dt.bfloat16
I32 = mybir.dt.int32
AF = mybir.ActivationFunctionType
ALU = mybir.AluOpType
AX = mybir.AxisListType

NCORES = 8
TOK = 2048
D = 1024
NT = TOK // 128
ST = 256
NST = TOK // ST
NJ = ST // 128
NCH = ST // 64
NPRE = (3 * TOK) // ST
EPS = 1e-6
BLK = 256
NB = (2 * TOK) // BLK + 32
NSLOT = NB * BLK
BIG = 1.0e30
SAME_ENGINE_SYNC = True


_BC_CACHE = {}


def _bc(e, val):
    if val not in _BC_CACHE:
        _BC_CACHE[val] = e.to_reg(val)
    return _BC_CACHE[val]


class Op_:
    __slots__ = ("eng", "fn", "preds", "dma_key", "idx", "level", "sem", "val", "cost")

    def __init__(self, eng, fn, preds, dma_key, idx, cost):
        self.eng = eng
        self.fn = fn
        self.preds = preds
        self.dma_key = dma_key
        self.idx = idx
        self.cost = cost
        self.level = 0.0
        self.sem = None
        self.val = 0


class Buf:
    ALL = []

    def __init__(self, name):
        self.name = name
        self.w = None
        self.r = []
        Buf.ALL.append(self)

    @staticmethod
    def reset_all():
        for b in Buf.ALL:
            b.w = None
            b.r = []


class Sched:
    ENGS = ("sync", "scalar", "vector", "gpsimd", "tensor")
    COST = {"sync": 2.0, "scalar": 1.0, "vector": 1.0, "gpsimd": 1.5, "tensor": 1.0}

    def __init__(self, nc, stack, tag):
        self.nc = nc
        self.stack = stack
        self.tag = tag
        self.ops = []
        self.sem = {e: stack.enter_context(nc.semaphore(f"{tag}_{e}")) for e in self.ENGS}
        self.dsem = {}
        self.final = []
        self.reorder = bool(int(os.environ.get("KREORDER", "1")))

    def _add(self, eng, fn, reads, writes, dma_key, cost):
        preds = set()
        for b in reads:
            if b.w is not None:
                preds.add(b.w)
        for b in writes:
            if b.w is not None:
                preds.add(b.w)
            preds.update(b.r)
        op = Op_(eng, fn, preds, dma_key, len(self.ops), cost)
        lv = 0.0
        for p in preds:
            lv = max(lv, p.level + p.cost)
        op.level = lv
        self.ops.append(op)
        for b in reads:
            b.r.append(op)
        for b in writes:
            b.w = op
            b.r = []
        return op

    def op(self, eng, fn, reads=(), writes=(), cost=None):
        return self._add(eng, fn, reads, writes, None, self.COST[eng] if cost is None else cost)

    def dma(self, eng, fn, reads=(), writes=(), key=None, final=False, cost=None):
        if key is None:
            key = writes[0].name if writes else reads[0].name
        if key not in self.dsem:
            self.dsem[key] = self.stack.enter_context(self.nc.semaphore(f"{self.tag}_d{len(self.dsem)}"))
        return self._add(eng, fn, reads, writes, key, 3.0 if cost is None else cost)

    def emit(self, block):
        order = sorted(self.ops, key=lambda o: (o.level, o.idx)) if self.reorder else list(self.ops)
        cnt = {e: 0 for e in self.ENGS}
        dcnt = {k: 0 for k in self.dsem}
        q = {e: [] for e in self.ENGS}
        waited = {e: {} for e in self.ENGS}
        for o in order:
            waits = {}
            wd = waited[o.eng]
            for p in o.preds:
                assert p.sem is not None, "predecessor not scheduled before successor"
                if p.dma_key is None and p.eng == o.eng and (o.eng == "tensor" or not SAME_ENGINE_SYNC):
                    continue
                k = id(p.sem)
                if wd.get(k, 0) < p.val:
                    if k not in waits or waits[k][1] < p.val:
                        waits[k] = (p.sem, p.val)
            for k, (sm, v) in waits.items():
                wd[k] = v
            if o.dma_key is None:
                cnt[o.eng] += 1
                o.sem, o.val = self.sem[o.eng], cnt[o.eng]
                inc = (o.sem, 1)
            else:
                dcnt[o.dma_key] += 16
                o.sem, o.val = self.dsem[o.dma_key], dcnt[o.dma_key]
                inc = (o.sem, 16)
            q[o.eng].append((list(waits.values()), o.fn, inc))
        fin = [(self.dsem[k], dcnt[k]) for k in self.dsem if dcnt[k] > 0]

        def run(eng, e):
            for waits, fn, inc in q[eng]:
                for sm, v in waits:
                    e.wait_ge(sm, v)
                ins = fn(e)
                ins.then_inc(inc[0], inc[1])
            if eng == "sync":
                for sm, v in fin:
                    e.wait_ge(sm, v)

        @block.sync
        def _(e):
            run("sync", e)

        @block.scalar
        def _(e):
            run("scalar", e)

        @block.vector
        def _(e):
            run("vector", e)

        @block.gpsimd
        def _(e):
            run("gpsimd", e)

        @block.tensor
        def _(e):
            run("tensor", e)


def build(debug=False, phases=2):
    _BC_CACHE.clear()
    nc = bass.Bass("TRN2", target_bir_lowering=False)
    dt = nc.dram_tensor
    x_d = dt("x", [TOK, D], F32, kind="ExternalInput").ap()
    xpre_d = dt("xpre", [NPRE * ST, D], F32, kind="ExternalInput").ap()
    win_d = dt("win", [128, 8, 3072], F32, kind="ExternalInput").ap()
    wout_d = dt("wout", [128, 8, 1024], F32, kind="ExternalInput").ap()
    wsT_d = dt("wsT", [128, 4, 128], F32, kind="ExternalInput").ap()
    bsbc_d = dt("bsbc", [128, 4, 128], F32, kind="ExternalInput").ap()
    small_d = dt("small", [128, 32], F32, kind="ExternalInput").ap()
    g2bc_d = dt("g2bc", [128, D], F32, kind="ExternalInput").ap()
    gfbc_d = dt("gfbc", [128, D], F32, kind="ExternalInput").ap()
    wr_d = dt("wr", [128, 8, 36], F32, kind="ExternalInput").ap()
    brbc_d = dt("brbc", [128, 36], F32, kind="ExternalInput").ap()
    consts_d = dt("consts", [128, 5, 128], F32, kind="ExternalInput").ap()
    rmask_d = dt("rmask", [128, 1024], F32, kind="ExternalInput").ap()
    blkthr_d = dt("blkthr", [128, NB], F32, kind="ExternalInput").ap()
    if phases >= 2:
        wg_d = dt("wg", [32 * 128, 4096], F32, kind="ExternalInput").ap()
        wu_d = dt("wu", [32 * 128, 4096], F32, kind="ExternalInput").ap()
        wd_d = dt("wd", [32 * 128, 4096], F32, kind="ExternalInput").ap()
    out_d = dt("out", [TOK, D], F32, kind="ExternalOutput").ap()
    hs_d = dt("hs", [TOK, D], F32, kind="Internal").ap()
    n2s_d = dt("n2s", [TOK, D], BF16, kind="Internal").ap()
    xs_d = dt("xs", [NSLOT, D], BF16, kind="Internal").ap()
    ys_d = dt("ys", [NSLOT, D], F32, kind="Internal").ap()
    if phases >= 2:
        wgb_d = dt("wgb", [32 * 128, 4096], BF16, kind="Internal").ap()
        wub_d = dt("wub", [32 * 128, 4096], BF16, kind="Internal").ap()
        wdb_d = dt("wdb", [32 * 128, 4096], BF16, kind="Internal").ap()
    if debug:
        dbg_h = dt("dbg_h", [TOK, D], F32, kind="ExternalOutput").ap()
        dbg_lg = dt("dbg_lg", [128, NT, 36], F32, kind="ExternalOutput").ap()
        dbg_s = dt("dbg_s", [128, 4, 128], F32, kind="ExternalOutput").ap()
        dbg_dec = dt("dbg_dec", [128, 4, NCH], F32, kind="ExternalOutput").ap()
        dbg_r = dt("dbg_r", [128, 8, NT], F32, kind="ExternalOutput").ap()

    with ExitStack() as gs:
        def sb(stack, name, shape, dtype):
            return stack.enter_context(nc.sbuf_tensor("s_" + name, shape, dtype))

        def ps(stack, name, shape, dtype):
            return stack.enter_context(nc.psum_tensor("p_" + name, shape, dtype))

        lg = sb(gs, "lg", [128, NT, 36], F32)
        cst = sb(gs, "cst", [128, 5, 128], F32)
        small = sb(gs, "small", [128, 32], F32)
        epsb = sb(gs, "epsb", [128, 1], F32)
        oneb = sb(gs, "oneb", [128, 1], F32)

        with ExitStack() as s1:
            S = Sched(nc, s1, "a")
            B = Buf
            win_bf = sb(s1, "win_bf", [128, 8, 3072], BF16)
            wout_bf = sb(s1, "wout_bf", [128, 8, 1024], BF16)
            wmT = sb(s1, "wmT", [128, 4, 128], BF16)
            bsbc = sb(s1, "bsbc", [128, 4, 128], F32)
            g2bc = sb(s1, "g2bc", [128, D], F32)
            wr = sb(s1, "wr", [128, 8, 36], F32)
            brbc = sb(s1, "brbc", [128, 36], F32)
            rmask = sb(s1, "rmask", [128, 4 * ST], F32)
            identb = sb(s1, "identb", [128, 128], BF16)
            onesb = sb(s1, "onesb", [128, 128], BF16)
            lbt = sb(s1, "lbt", [128, 12], F32)
            NXS = 4
            xt = [sb(s1, f"xt{i}", [128, D], F32) for i in range(NXS)]
            stat = sb(s1, "stat", [128, NT + 2, 8], F32)
            xn = [sb(s1, f"xn{i}", [128, D], BF16) for i in range(2)]
            n1T = [sb(s1, f"n1T{i}", [128, 8, ST], BF16) for i in range(2)]
            guT = sb(s1, "guT", [128, 4, ST], BF16)
            sgT = sb(s1, "sgT", [128, 4, ST], BF16)
            dec = sb(s1, "dec", [128, 4, NCH], F32)
            qeT = sb(s1, "qeT", [128, 4, ST], BF16)
            keT = sb(s1, "keT", [128, 4, ST], BF16)
            qdT = sb(s1, "qdT", [128, 4, ST], BF16)
            kdtm = [sb(s1, f"kdtm{i}", [128, 4, NJ, 128], BF16) for i in range(2)]
            gv = sb(s1, "gv", [128, 512], F32)
            gsq = sb(s1, "gsq", [128, 512], F32)
            vst = sb(s1, "vst", [128, NJ, 12], F32)
            vhn = sb(s1, "vhn", [128, NJ, 512], BF16)
            vi = sb(s1, "vi", [128, NJ, 512], BF16)
            tmpA = [sb(s1, f"tmpA{i}", [128, 128], F32) for i in range(2)]
            S32 = sb(s1, "S32", [128, 4, 128], F32)
            Sbf4 = [sb(s1, f"Sbf4_{i}", [128, 4, 128], BF16) for i in range(3)]
            n2b2 = [sb(s1, f"n2b{i}", [128, D], BF16) for i in range(2)]
            pT = ps(s1, "pT", [128, 1024], BF16)
            pK = ps(s1, "pK", [128, 1024], BF16)
            pP = [ps(s1, f"pP{i}", [128, 512], F32) for i in range(2)]
            pG = ps(s1, "pG", [128, 512], F32)
            pS = ps(s1, "pS", [128, 512], F32)
            pO = ps(s1, "pO", [128, 512], F32)
            pU = ps(s1, "pU", [128, 512], F32)

            b_cst = B("cst"); b_small = B("small"); b_lg = B("lg")
            b_win = [B(f"win{k}") for k in range(8)]
            b_wout = B("wout")
            b_wstage = B("wstage")
            b_wsT = B("wsT"); b_wmT = B("wmT"); b_bsbc = B("bsbc"); b_g2bc = B("g2bc")
            b_wr = B("wr"); b_brbc = B("brbc"); b_rmask = B("rmask")
            b_identb = B("identb"); b_onesb = B("onesb"); b_lbt = B("lbt")
            b_xt = [B(f"xt{i}") for i in range(NXS)]
            b_sqj = B("sqj")
            b_stat = [B(f"stat{t}") for t in range(NT + 2)]
            b_xn = [B(f"xn{i}") for i in range(2)]
            b_n1T = [[B(f"n1T{i}_{j}") for j in range(NJ)] for i in range(2)]
            b_guT = [B(f"guT{g}") for g in range(4)]
            b_sgT = [B(f"sgT{g}") for g in range(4)]
            b_qs = [B(f"qs{i}") for i in range(2)]
            b_fk = [B(f"fk{i}") for i in range(2)]
            b_l1 = [B(f"l1{i}") for i in range(2)]
            b_bT = [B(f"bT{i}") for i in range(2)]
            b_d3 = [B(f"d3{i}") for i in range(2)]
            b_E12 = [B(f"E12{i}") for i in range(2)]
            b_kdh = [B(f"kdh{i}") for i in range(2)]
            b_dec = [B(f"dec{h}") for h in range(4)]
            b_qeT = [B(f"qeT{h}") for h in range(4)]
            b_keT = [B(f"keT{h}") for h in range(4)]
            b_qdT = [B(f"qdT{h}") for h in range(4)]
            b_kdtm = [B(f"kdtm{h}") for h in range(4)]
            b_gv = B("gv")
            b_gsq = B("gsq")
            b_vst = [B(f"vst{j}") for j in range(NJ)]
            b_vhn = [B(f"vhn{j}") for j in range(NJ)]
            b_vi = [B(f"vi{j}") for j in range(NJ)]
            b_tmpA = [B(f"tmpA{i}") for i in range(2)]
            b_scb = [B(f"scb{i}") for i in range(4)]
            b_S32 = [B(f"S32{h}") for h in range(4)]
            b_Sbf4 = [B(f"Sbf4_{i}") for i in range(3)]
            b_o32 = [B(f"o32{h}") for h in range(4)]
            b_osq = B("osq"); b_osd = B("osd"); b_ors = B("ors"); b_otmp = B("otmp")
            b_abT = [[B(f"abT{c}_{j}") for j in range(NJ)] for c in range(8)]
            b_hx = [B(f"hx{i}") for i in range(2)]
            b_n2f = B("n2f")
            b_n2b2 = [B("n2b0"), B("n2b1")]
            b_n2T = B("n2T")
            b_pT = B("pT"); b_pK = [B("pK0"), B("pK1")]
            b_pP = [B("pP0"), B("pP1")]
            b_pG = B("pG")
            b_pS = [B(f"pS{i}") for i in range(4)]
            b_pO = [B(f"pO{i}") for i in range(4)]
            b_pU = [B(f"pU{i}") for i in range(4)]
            b_hs = B("hs_d"); b_n2s = B("n2s_d")
            b_dbg = B("dbg")

            s1a = ExitStack()
            wstage = sb(s1a, "wstage", [128, 3072], F32)
            wsT = sb(s1a, "wsT", [128, 4, 128], F32)
            sg4 = [sb(s1a, f"sg4{i}", [128, 4, ST], F32) for i in range(2)]
            vip = [sb(s1a, f"vip{i}", [128, NJ, 512], BF16) for i in range(2)]
            l14 = sb(s1a, "l14", [128, 4, ST], F32)
            k4 = sb(s1a, "k4", [128, 4, ST], F32)
            bT4 = sb(s1a, "bT4", [128, 4, ST], F32)
            d34 = sb(s1a, "d34", [128, 4, ST], F32)
            kd4 = sb(s1a, "kd4", [128, 4, ST], BF16)
            blp = sb(s1a, "blp", [128, 4, NCH], F32)
            decp = sb(s1a, "decp", [128, 4, NCH], F32)
            b_sg4 = [B("sg40"), B("sg41")]; b_vip = [B("vip0"), B("vip1")]
            b_l14 = B("l14"); b_k4 = B("k4"); b_bT4 = B("bT4"); b_d34 = B("d34"); b_kd4 = B("kd4")
            b_blp = B("blp"); b_decp = B("decp")
            S.dma("sync", lambda e: e.dma_start(out=cst[:], in_=consts_d[:, :, :]), writes=[b_cst])
            S.dma("sync", lambda e: e.dma_start(out=small[:], in_=small_d[:, :]), writes=[b_small])
            S.dma("sync", lambda e: e.dma_start(out=wsT[:], in_=wsT_d[:, :, :]), writes=[b_wsT])
            S.dma("sync", lambda e: e.dma_start(out=bsbc[:], in_=bsbc_d[:, :, :]), writes=[b_bsbc])
            S.dma("sync", lambda e: e.dma_start(out=rmask[:], in_=rmask_d[:, 0:4 * ST]), writes=[b_rmask])
            S.dma("scalar", lambda e: e.dma_start(out=g2bc[:], in_=g2bc_d[:, :]), writes=[b_g2bc])
            S.dma("scalar", lambda e: e.dma_start(out=wr[:], in_=wr_d[:, :, :]), writes=[b_wr])
            S.dma("scalar", lambda e: e.dma_start(out=brbc[:], in_=brbc_d[:, :]), writes=[b_brbc])
            for k in range(8):
                S.dma("gpsimd", lambda e, k=k: e.dma_start(out=wout_bf[:, k, :], in_=wout_d[:, k, :]),
                      writes=[b_wout], key="wout")
            b_epsb = B("epsb")
            S.op("vector", lambda e: e.memset(epsb[:], EPS), writes=[b_epsb])
            S.op("vector", lambda e: e.memset(oneb[:], 1.0), writes=[b_epsb])
            S.op("vector", lambda e: e.tensor_copy(out=identb[:], in_=cst[:, 0, :]), reads=[b_cst], writes=[b_identb])
            S.op("vector", lambda e: e.tensor_copy(out=onesb[:], in_=cst[:, 4, :]), reads=[b_cst], writes=[b_onesb])
            S.op("vector", lambda e: e.tensor_tensor(out=wmT[:], in0=wsT[:], in1=cst[:, 2:3, :].to_broadcast([128, 4, 128]),
                                                     op=ALU.mult), reads=[b_cst, b_wsT], writes=[b_wmT])
            lbraw = small[:, 4:12].rearrange("p (h r) -> p h r", r=2)
            S.op("vector", lambda e: e.tensor_tensor(out=lbt[:, 8:12], in0=lbraw[:, :, 0], in1=lbraw[:, :, 1], op=ALU.subtract),
                 reads=[b_small], writes=[b_lbt])
            S.op("scalar", lambda e: e.activation(out=lbt[:, 0:4], in_=lbt[:, 8:12], func=AF.Sigmoid),
                 reads=[b_lbt], writes=[b_lbt])
            S.op("vector", lambda e: e.tensor_scalar(out=lbt[:, 4:8], in0=lbt[:, 0:4], scalar1=-1.0, scalar2=1.0,
                                                     op0=ALU.mult, op1=ALU.add), reads=[b_lbt], writes=[b_lbt])
            S.op("vector", lambda e: e.tensor_scalar(out=lbt[:, 8:12], in0=lbt[:, 4:8], scalar1=-1.0, scalar2=None, op0=ALU.mult),
                 reads=[b_lbt], writes=[b_lbt])
            S.op("vector", lambda e: e.memset(S32[:], 0.0), writes=b_S32)
            S.op("vector", lambda e: e.memset(kdtm[0][:], 0.0), writes=b_kdtm)
            S.op("vector", lambda e: e.memset(kdtm[1][:], 0.0), writes=b_kdtm)
            S.op("vector", lambda e: e.memset(Sbf4[0][:], 0.0), writes=[b_Sbf4[0]])
            for k in range(8):
                S.dma("sync" if k % 2 == 0 else "scalar",
                      lambda e, k=k: e.dma_start(out=wstage[:], in_=win_d[:, k, :]), writes=[b_wstage])
                S.op("scalar", lambda e, k=k: e.activation(out=win_bf[:, k, 0:1536], in_=wstage[:, 0:1536], func=AF.Copy,
                                                           scale=small[:, 16 + k:17 + k]),
                     reads=[b_wstage, b_small], writes=[b_win[k]])
                S.op("vector", lambda e, k=k: e.tensor_scalar(out=win_bf[:, k, 1536:3072], in0=wstage[:, 1536:3072],
                                                              scalar1=small[:, 16 + k:17 + k], scalar2=None, op0=ALU.mult),
                     reads=[b_wstage, b_small], writes=[b_win[k]])

            sbf_cur = [0, 0, 0, 0]
            precast = []
            if phases >= 2:
                for ex in range(32):
                    for src, dst in ((wg_d, wgb_d), (wu_d, wub_d), (wd_d, wdb_d)):
                        precast.append((src, dst, ex))

            def emit_precast(n, dep):
                for _ in range(n):
                    if not precast:
                        return
                    src, dst, ex = precast.pop(0)
                    S.dma("gpsimd", lambda e, src=src, dst=dst, ex=ex: e.dma_start(
                        out=dst[ex * 128:(ex + 1) * 128, :].rearrange("r (a c) -> r a c", c=2048),
                        in_=src[ex * 128:(ex + 1) * 128, :].rearrange("r (a c) -> r a c", c=2048)),
                        reads=[dep], writes=[B(f"pc{len(precast)}")], key=f"pc{len(precast) % 4}")

            def rms_stats(tile_t, src, b_src, col, junk=None, b_junk=None):
                S.op("scalar", lambda e: e.activation(out=junk[:], in_=src, func=AF.Square,
                                                      accum_out=stat[:, tile_t, col:col + 1]),
                     reads=[b_src], writes=[b_junk, b_stat[tile_t]])
                S.op("scalar", lambda e: e.activation(out=stat[:, tile_t, col + 1:col + 2], in_=stat[:, tile_t, col:col + 1],
                                                      func=AF.Ln, scale=1.0 / D, bias=epsb[:, 0:1]),
                     reads=[b_stat[tile_t], b_epsb], writes=[b_stat[tile_t]])
                S.op("scalar", lambda e: e.activation(out=stat[:, tile_t, col + 2:col + 3], in_=stat[:, tile_t, col + 1:col + 2],
                                                      func=AF.Exp, scale=-0.5),
                     reads=[b_stat[tile_t]], writes=[b_stat[tile_t]])

            pp_i = [0]

            def next_pp():
                i = pp_i[0]
                pp_i[0] ^= 1
                return i

            gcount = [0]

            def super_tile(st, pre, last_pre=False):
                nb = gcount[0] % 2
                gcount[0] += 1
                src_d = xpre_d if pre else x_d
                abT = abT2[st % 2]
                b_abT = b_abT2[st % 2]
                sgT_ = sgT2[st % 2]
                b_sgT_ = b_sgT2[st % 2]
                for j in range(NJ):
                    t = st * NJ + j
                    xs_i = t % NXS
                    S.dma("sync", lambda e, t=t, xs_i=xs_i: e.dma_start(out=xt[xs_i][:], in_=src_d[t * 128:(t + 1) * 128, :]),
                          writes=[b_xt[xs_i]])
                    ts = (NT + t % 2) if pre else t
                    xi = t % 2
                    rms_stats(ts, xt[xs_i][:], b_xt[xs_i], 0, xn[xi], b_xn[xi])
                    S.op("vector", lambda e, ts=ts, xs_i=xs_i, xi=xi: e.tensor_scalar(out=xn[xi][:], in0=xt[xs_i][:],
                                                                                     scalar1=stat[:, ts, 2:3], scalar2=None,
                                                                                     op0=ALU.mult),
                         reads=[b_xt[xs_i], b_stat[ts]], writes=[b_xn[xi]])

                    def tr(e, xi=xi):
                        ins = None
                        for k in range(8):
                            ins = e.transpose(out=pT[:, k * 128:(k + 1) * 128], in_=xn[xi][:, k * 128:(k + 1) * 128],
                                              identity=identb[:])
                        return ins
                    S.op("tensor", tr, reads=[b_xn[xi], b_identb], writes=[b_pT])
                    S.op("vector", lambda e, nb=nb, j=j: e.tensor_copy(
                        out=n1T[nb][:, :, j * 128:(j + 1) * 128], in_=pT[:].rearrange("p (k t) -> p k t", t=128)),
                        reads=[b_pT], writes=[b_n1T[nb][j]])

                def proj_fm(col0, pi, nb=nb):
                    def f(e):
                        ins = None
                        for k in range(8):
                            ins = e.matmul(pP[pi][:, 0:ST], lhsT=win_bf[:, k, col0:col0 + 128], rhs=n1T[nb][:, k, :],
                                           start=(k == 0), stop=(k == 7))
                        return ins
                    S.op("tensor", f, reads=b_win + b_n1T[nb], writes=[b_pP[pi]])

                def proj_tm(col0, j, pi, nb=nb):
                    def f(e):
                        ins = None
                        for k in range(8):
                            ins = e.matmul(pP[pi][:], lhsT=n1T[nb][:, k, j * 128:(j + 1) * 128], rhs=win_bf[:, k, col0:col0 + 512],
                                           start=(k == 0), stop=(k == 7))
                        return ins
                    S.op("tensor", f, reads=b_win + [b_n1T[nb][j]], writes=[b_pP[pi]])

                KPCUT = int(os.environ.get("KPCUT", "9"))
                if pre and KPCUT <= 0:
                    return
                if not pre:
                    emit_precast(2, b_n1T[nb][0])
                for g in range(0 if pre else 4):
                    pi = next_pp()
                    proj_fm(g * 128, pi)
                    S.op("scalar", lambda e, g=g, pi=pi: e.activation(out=guT[:, g, :], in_=pP[pi][:, 0:ST], func=AF.Gelu),
                         reads=[b_pP[pi]], writes=[b_guT[g]])
                for j in range(NJ):
                    if pre:
                        pi = next_pp()
                        proj_tm(2048, j, pi)
                        S.op("scalar", lambda e, pi=pi, j=j: e.activation(out=vi[:, j, :], in_=pP[pi][:], func=AF.Copy),
                             reads=[b_pP[pi]], writes=[b_vi[j]])
                        continue
                    pi = next_pp()
                    proj_tm(512, j, pi)
                    S.op("scalar", lambda e, pi=pi: e.activation(out=gv[:], in_=pP[pi][:], func=AF.Gelu),
                         reads=[b_pP[pi]], writes=[b_gv])
                    S.op("vector", lambda e: e.tensor_tensor(out=gsq[:], in0=gv[:], in1=gv[:], op=ALU.mult),
                         reads=[b_gv], writes=[b_gsq])
                    S.op("vector", lambda e, j=j: e.tensor_reduce(out=vst[:, j, 0:4], in_=gsq[:].rearrange("p (g c) -> p g c", c=128),
                                                                 axis=AX.X, op=ALU.add),
                         reads=[b_gsq], writes=[b_vst[j]])
                    S.op("scalar", lambda e, j=j: e.activation(out=vst[:, j, 4:8], in_=vst[:, j, 0:4], func=AF.Ln,
                                                               scale=1.0 / 128, bias=epsb[:, 0:1]),
                         reads=[b_vst[j], b_epsb], writes=[b_vst[j]])
                    S.op("scalar", lambda e, j=j: e.activation(out=vst[:, j, 8:12], in_=vst[:, j, 4:8], func=AF.Exp, scale=-0.5),
                         reads=[b_vst[j]], writes=[b_vst[j]])
                    S.op("vector", lambda e, j=j: e.tensor_tensor(
                        out=vhn[:, j, :].rearrange("p (g c) -> p g c", c=128),
                        in0=gv[:].rearrange("p (g c) -> p g c", c=128),
                        in1=vst[:, j, 8:12].rearrange("p (g o) -> p g o", o=1).to_broadcast([128, 4, 128]), op=ALU.mult),
                        reads=[b_gv, b_vst[j]], writes=[b_vhn[j]])
                    pi = next_pp()
                    proj_tm(2048, j, pi)
                    S.op("scalar", lambda e, pi=pi, j=j: e.activation(out=vi[:, j, :], in_=pP[pi][:], func=AF.Copy),
                         reads=[b_pP[pi]], writes=[b_vi[j]])
                if pre and KPCUT <= 1:
                    return
                if not pre:
                    emit_precast(2, b_vi[0])
                for j in range(0 if pre else NJ):
                    def gm(e, j=j):
                        ins = None
                        for g in range(4):
                            ins = e.matmul(pG[:, g * 128:(g + 1) * 128], lhsT=vhn[:, j, g * 128:(g + 1) * 128], rhs=wmT[:, g, :],
                                           start=True, stop=True)
                        return ins
                    S.op("tensor", gm, reads=[b_vhn[j], b_wmT], writes=[b_pG])
                    for g in range(4):
                        ti = g % 2
                        S.op("vector", lambda e, g=g, ti=ti: e.scalar_tensor_tensor(
                            out=tmpA[ti][:], in0=pG[:, g * 128:(g + 1) * 128], scalar=small[:, g:g + 1], in1=bsbc[:, g, :],
                            op0=ALU.mult, op1=ALU.add), reads=[b_pG, b_small, b_bsbc], writes=[b_tmpA[ti]])
                        S.op("gpsimd", lambda e, g=g, ti=ti, j=j: e.tensor_tensor(
                            out=abT[:, g, j * 128:(j + 1) * 128], in0=tmpA[ti][:], in1=guT[:, g, j * 128:(j + 1) * 128], op=ALU.mult),
                            reads=[b_tmpA[ti], b_guT[g]], writes=[b_abT[g][j]])
                def proj2(col_base, bank, b_bank, bk, nb=nb):
                    def f(e):
                        ins = None
                        for hh in range(2):
                            h = bk * 2 + hh
                            for k in range(8):
                                ins = e.matmul(bank[:, hh * ST:(hh + 1) * ST], lhsT=win_bf[:, k, col_base + h * 128:col_base + (h + 1) * 128],
                                               rhs=n1T[nb][:, k, :], start=(k == 0), stop=(k == 7))
                        return ins
                    S.op("tensor", f, reads=b_win + b_n1T[nb], writes=b_bank)

                def v2(bank):
                    return bank[:, 0:2 * ST].rearrange("p (h t) -> p h t", t=ST)
                fbanks = ((pP[0], [b_pP[0]]), (pP[1], [b_pP[1]]))
                qbanks = fbanks
                gbanks = fbanks
                for bk in range(2):
                    bank, bb = fbanks[bk]
                    proj2(1536, bank, bb, bk)
                    S.op("scalar", lambda e, bk=bk, bank=bank: e.activation(out=sg4m[:, bk * 2:(bk + 1) * 2, :], in_=v2(bank), func=AF.Exp,
                                                                          scale=-1.0), reads=bb, writes=[b_sg4m])
                S.op("scalar", lambda e: e.activation(out=sg4m[:], in_=sg4m[:], func=AF.Ln, bias=oneb[:, 0:1]),
                     reads=[b_sg4m], writes=[b_sg4m])
                S.op("scalar", lambda e: e.activation(out=sg4m[:], in_=sg4m[:], func=AF.Exp, scale=-1.0),
                     reads=[b_sg4m], writes=[b_sg4m])
                for bk in range(2):
                    bank, bb = qbanks[bk]
                    proj2(1024, bank, bb, bk)
                    S.op("scalar", lambda e, bk=bk, bank=bank: e.activation(out=qs4[:, bk * 2:(bk + 1) * 2, :], in_=v2(bank), func=AF.Silu),
                         reads=bb, writes=[b_qs4])
                for bk in range(2):
                    bank, bb = gbanks[bk]
                    proj2(2560, bank, bb, bk)
                    S.op("scalar", lambda e, bk=bk, bank=bank: e.activation(out=sgT_[:, bk * 2:(bk + 1) * 2, :], in_=v2(bank), func=AF.Silu),
                         reads=bb, writes=[b_sgT_[bk * 2], b_sgT_[bk * 2 + 1]])
                for h in range(4):
                    S.op("scalar", lambda e, h=h: e.activation(out=l14m[:, h, :], in_=sg4m[:, h, :], func=AF.Ln,
                                                               scale=lbt[:, 4 + h:5 + h], bias=lbt[:, h:h + 1]),
                         reads=[b_sg4m, b_lbt], writes=[b_l14m])
                for h in range(4):
                    S.op("vector", lambda e, h=h: e.tensor_scalar(
                        out=k4m[:, h, :], in0=sg4m[:, h, :], scalar1=lbt[:, 8 + h:9 + h], scalar2=lbt[:, 4 + h:5 + h],
                        op0=ALU.mult, op1=ALU.add), reads=[b_sg4m, b_lbt], writes=[b_k4m])
                S.op("vector", lambda e: e.tensor_tensor_scan(out=bT4m[:].rearrange("p h t -> p (h t)"), data0=rmask[:],
                                                              data1=l14m[:].rearrange("p h t -> p (h t)"), initial=0.0,
                                                              op0=ALU.mult, op1=ALU.add),
                     reads=[b_l14m, b_rmask], writes=[b_bT4m])
                bvm = bT4m[:].rearrange("p h (c t) -> p (h c) t", t=64)
                S.op("vector", lambda e: e.tensor_tensor(
                    out=l14m[:].rearrange("p h (c t) -> p (h c) t", t=64), in0=bvm,
                    in1=bvm[:, :, 31:32].to_broadcast([128, 4 * NCH, 64]), op=ALU.subtract),
                    reads=[b_bT4m], writes=[b_l14m])
                S.op("vector", lambda e: e.tensor_tensor(
                    out=d34m[:].rearrange("p h (c t) -> p (h c) t", t=64), in0=bvm,
                    in1=bvm[:, :, 63:64].to_broadcast([128, 4 * NCH, 64]), op=ALU.subtract),
                    reads=[b_bT4m], writes=[b_d34m])
                S.op("vector", lambda e: e.tensor_copy(out=blm[:].rearrange("p h c -> p (h c)"), in_=bvm[:, :, 63]),
                     reads=[b_bT4m], writes=[b_blm])
                S.op("scalar", lambda e: e.activation(out=E12m[:], in_=l14m[:], func=AF.Exp), reads=[b_l14m], writes=[b_E12m])
                S.op("vector", lambda e: e.tensor_tensor(out=qeT[:], in0=qs4[:], in1=E12m[:], op=ALU.mult),
                     reads=[b_qs4, b_E12m], writes=b_qeT)
                S.op("scalar", lambda e: e.activation(out=E12m[:], in_=l14m[:], func=AF.Exp, scale=-1.0), reads=[b_l14m], writes=[b_E12m])
                S.op("vector", lambda e: e.tensor_tensor(out=keT[:], in0=k4m[:], in1=E12m[:], op=ALU.mult),
                     reads=[b_k4m, b_E12m], writes=b_keT)
                S.op("scalar", lambda e: e.activation(out=d34m[:], in_=d34m[:], func=AF.Exp, scale=-1.0), reads=[b_d34m], writes=[b_d34m])
                S.op("gpsimd", lambda e: e.tensor_tensor(out=kd4m[:], in0=k4m[:], in1=d34m[:], op=ALU.mult),
                     reads=[b_k4m, b_d34m], writes=[b_kd4m])
                S.op("scalar", lambda e: e.activation(out=decm[:], in_=blm[:], func=AF.Exp), reads=[b_blm], writes=[b_decm])
                S.op("scalar", lambda e: e.activation(out=bT4m[:], in_=bT4m[:], func=AF.Exp), reads=[b_bT4m], writes=[b_bT4m])
                S.op("vector", lambda e: e.tensor_tensor(out=qdT[:], in0=qs4[:], in1=bT4m[:], op=ALU.mult),
                     reads=[b_qs4, b_bT4m], writes=b_qdT)

                def trkm(e):
                    ins = None
                    for h in range(4):
                        for j in range(NJ):
                            c0_ = (h * NJ + j) * 128
                            ins = e.transpose(out=pK[:, c0_:c0_ + 128], in_=kd4m[:, h, j * 128:(j + 1) * 128], identity=identb[:])
                    return ins
                S.op("tensor", trkm, reads=[b_kd4m, b_identb], writes=b_pK)
                S.op("scalar", lambda e: e.activation(
                    out=kdtm[0][0:64, :, :, :], in_=pK[0:64, 0:4 * NJ * 128].rearrange("p (h j d) -> p h j d", j=NJ, d=128), func=AF.Copy),
                    reads=b_pK, writes=b_kdtm)
                S.op("scalar", lambda e: e.activation(
                    out=kdtm[1][64:128, :, :, :], in_=pK[64:128, 0:4 * NJ * 128].rearrange("p (h j d) -> p h j d", j=NJ, d=128), func=AF.Copy),
                    reads=b_pK, writes=b_kdtm)
                emit_precast(2, b_kd4m)
                def v4(bank):
                    return bank[:].rearrange("p (h e) -> p h e", e=128)
                for j in range(NJ):
                    c0 = sbf_cur[0]
                    ca = (c0 + 1) % 3
                    cb = (c0 + 2) % 3
                    sj = j % 2

                    def scf(e, j=j):
                        ins = None
                        for h in range(4):
                            ins = e.matmul(pS[:, h * 128:(h + 1) * 128], lhsT=keT[:, h, j * 128:(j + 1) * 128],
                                           rhs=qeT[:, h, j * 128:(j + 1) * 128], start=True, stop=True)
                        return ins
                    S.op("tensor", scf, reads=b_keT + b_qeT, writes=b_pS)

                    def umf(e, j=j, half=0, bank=pU):
                        ins = None
                        for h in range(4):
                            ins = e.matmul(bank[:, h * 128:(h + 1) * 128], lhsT=kdtm[half][:, h, j, :],
                                           rhs=vi[:, j, h * 128:(h + 1) * 128], start=True, stop=True)
                        return ins
                    S.op("tensor", umf, reads=b_kdtm + [b_vi[j]], writes=b_pU)
                    S.op("vector", lambda e, sj=sj: e.tensor_tensor(out=scb4[sj][:], in0=v4(pS), in1=cst[:, 1:2, :].to_broadcast([128, 4, 128]),
                                                                   op=ALU.mult), reads=b_pS + [b_cst], writes=[b_scb4[sj]])
                    for h in range(4):
                        S.op("vector", lambda e, j=j, h=h: e.scalar_tensor_tensor(
                            out=S32[:, h, :], in0=S32[:, h, :], scalar=decm[:, h, 2 * j:2 * j + 1], in1=pU[:, h * 128:(h + 1) * 128],
                            op0=ALU.mult, op1=ALU.add), reads=[b_S32[h], b_decm] + b_pU, writes=[b_S32[h]])
                    S.op("scalar", lambda e, ca=ca: e.activation(out=Sbf4[ca][:], in_=S32[:], func=AF.Copy),
                         reads=b_S32, writes=[b_Sbf4[ca]])

                    def ogf(e, j=j, sj=sj, c0=c0, ca=ca):
                        ins = None
                        for h in range(4):
                            e.matmul(pO[:, h * 128:(h + 1) * 128], lhsT=vi[:, j, h * 128:(h + 1) * 128], rhs=scb4[sj][:, h, :],
                                     start=True, stop=False)
                            e.matmul(pO[:, h * 128:h * 128 + 64], lhsT=Sbf4[c0][:, h, :], rhs=qdT[:, h, j * 128:j * 128 + 64],
                                     start=False, stop=False)
                            ins = e.matmul(pO[:, h * 128 + 64:(h + 1) * 128], lhsT=Sbf4[ca][:, h, :],
                                           rhs=qdT[:, h, j * 128 + 64:(j + 1) * 128], start=False, stop=True)
                        return ins
                    S.op("tensor", ogf, reads=[b_vi[j], b_scb4[sj], b_Sbf4[c0], b_Sbf4[ca]] + b_qdT, writes=b_pO)
                    S.op("tensor", lambda e, j=j: umf(e, j, 1, pU), reads=b_kdtm + [b_vi[j]], writes=b_pU)
                    S.op("scalar", lambda e, j=j: e.activation(out=o32[:, :, j * 128:(j + 1) * 128], in_=v4(pO), func=AF.Copy),
                         reads=b_pO, writes=b_o32)
                    for h in range(4):
                        S.op("vector", lambda e, j=j, h=h: e.scalar_tensor_tensor(
                            out=S32[:, h, :], in0=S32[:, h, :], scalar=decm[:, h, 2 * j + 1:2 * j + 2], in1=pU[:, h * 128:(h + 1) * 128],
                            op0=ALU.mult, op1=ALU.add), reads=[b_S32[h], b_decm] + b_pU, writes=[b_S32[h]])
                    S.op("scalar", lambda e, cb=cb: e.activation(out=Sbf4[cb][:], in_=S32[:], func=AF.Copy),
                         reads=b_S32, writes=[b_Sbf4[cb]])
                    sbf_cur[0] = cb
                emit_precast(2 - (st % 2), b_o32[0])
                S.op("scalar", lambda e: e.activation(out=osq4[:], in_=o32[:], func=AF.Square), reads=b_o32, writes=[b_osq4])
                backb = ((pS, b_pS), (pO, b_pO))
                for bk in range(2):
                    bkb, bbb = backb[bk]
                    S.op("tensor", lambda e, bk=bk, bkb=bkb: e.matmul(bkb[:, 0:2 * ST], lhsT=onesb[:],
                                                                      rhs=osq4[:, bk * 2:(bk + 1) * 2, :], start=True, stop=True),
                         reads=[b_onesb, b_osq4], writes=bbb)
                    S.op("scalar", lambda e, bk=bk, bkb=bkb: e.activation(out=l14m[:, bk * 2:(bk + 1) * 2, :], in_=v2(bkb), func=AF.Ln,
                                                                          scale=1.0 / 128, bias=epsb[:, 0:1]),
                         reads=bbb + [b_epsb], writes=[b_l14m])
                S.op("scalar", lambda e: e.activation(out=k4m[:], in_=l14m[:], func=AF.Exp, scale=-0.5), reads=[b_l14m], writes=[b_k4m])
                S.op("vector", lambda e: e.tensor_tensor(
                    out=d34m[:], in0=o32[:], in1=small[:, 12:16].rearrange("p (h o) -> p h o", o=1).to_broadcast([128, 4, ST]), op=ALU.mult),
                    reads=b_o32 + [b_small], writes=[b_d34m])
                S.op("vector", lambda e: e.tensor_tensor(out=d34m[:], in0=d34m[:], in1=k4m[:], op=ALU.mult),
                     reads=[b_d34m, b_k4m], writes=[b_d34m])
                S.op("gpsimd", lambda e: e.tensor_tensor(out=abT[:, 4:8, :], in0=d34m[:], in1=sgT_[:], op=ALU.mult),
                     reads=[b_d34m] + b_sgT_, writes=[x_ for c_ in range(4, 8) for x_ in b_abT[c_]])
                for j in range(NJ):
                    t = st * NJ + j
                    xs_i = t % NXS
                    hi = t % 2
                    for half in range(2):
                        bkb, bbb = backb[half]

                        def op_(e, j=j, half=half, bkb=bkb, abT=abT):
                            ins = None
                            for kc in range(8):
                                ins = e.matmul(bkb[:], lhsT=abT[:, kc, j * 128:(j + 1) * 128],
                                               rhs=wout_bf[:, kc, half * 512:(half + 1) * 512], start=(kc == 0), stop=(kc == 7))
                            return ins
                        S.op("tensor", op_, reads=[b_abT[c][j] for c in range(8)] + [b_wout], writes=bbb)
                        S.op("vector", lambda e, half=half, bkb=bkb, hi=hi, xs_i=xs_i: e.tensor_tensor(
                            out=hx[hi][:, half * 512:(half + 1) * 512], in0=bkb[:], in1=xt[xs_i][:, half * 512:(half + 1) * 512],
                            op=ALU.add), reads=bbb + [b_xt[xs_i]], writes=[b_hx[hi]])
                    S.dma("sync", lambda e, t=t, hi=hi: e.dma_start(out=hs_d[t * 128:(t + 1) * 128, :], in_=hx[hi][:]),
                          reads=[b_hx[hi]], writes=[B(f"hs_t{t}")], key=f"hs{hi}")
                    if debug:
                        S.dma("sync", lambda e, t=t, hi=hi: e.dma_start(out=dbg_h[t * 128:(t + 1) * 128, :], in_=hx[hi][:]),
                              reads=[b_hx[hi]], writes=[b_dbg], key="dbg", final=True)
                    rms_stats(t, hx[hi][:], b_hx[hi], 3, n2f, b_n2f)
                    n2b = n2b2[t % 2]
                    b_n2b = b_n2b2[t % 2]
                    S.op("vector", lambda e, t=t, hi=hi: e.scalar_tensor_tensor(
                        out=n2f[:], in0=hx[hi][:], scalar=stat[:, t, 5:6], in1=g2bc[:], op0=ALU.mult, op1=ALU.mult),
                        reads=[b_hx[hi], b_stat[t], b_g2bc], writes=[b_n2f])
                    S.op("scalar", lambda e, n2b=n2b: e.activation(out=n2b[:], in_=n2f[:], func=AF.Copy),
                         reads=[b_n2f], writes=[b_n2b])
                    S.dma("scalar", lambda e, t=t, n2b=n2b: e.dma_start(out=n2s_d[t * 128:(t + 1) * 128, :], in_=n2b[:]),
                          reads=[b_n2b], writes=[B(f"n2s_t{t}")], key=f"n2s{t % 2}")
                    trb = ((pU, b_pU), (pS, b_pS))
                    for half in range(2):
                        bkb, bbb = trb[half]

                        def tr2(e, half=half, bkb=bkb):
                            ins = None
                            for kk in range(4):
                                k = half * 4 + kk
                                ins = e.transpose(out=bkb[:, kk * 128:(kk + 1) * 128], in_=n2f[:, k * 128:(k + 1) * 128],
                                                  identity=cst[:, 0, :])
                            return ins
                        S.op("tensor", tr2, reads=[b_n2f, b_cst], writes=bbb)
                        S.op("vector", lambda e, half=half, bkb=bkb: e.tensor_copy(
                            out=n2T[:, half * 4:(half + 1) * 4, :], in_=bkb[:].rearrange("p (k t) -> p k t", t=128)),
                            reads=bbb, writes=[b_n2T])

                    def rt(e):
                        ins = None
                        for k in range(8):
                            ins = e.matmul(pO[:, 0:36], lhsT=n2T[:, k, :], rhs=wr[:, k, :], start=(k == 0), stop=(k == 7))
                        return ins
                    S.op("tensor", rt, reads=[b_n2T, b_wr], writes=b_pO)
                    S.op("vector", lambda e, t=t: e.tensor_tensor(out=lg[:, t, :], in0=pO[:, 0:36], in1=brbc[:], op=ALU.add),
                         reads=b_pO + [b_brbc], writes=[b_lg])
            def pre_A(pst, i2):
                nb = gcount[0] % 2
                gcount[0] += 1
                for j in range(NJ):
                    t = pst * NJ + j
                    xs_i = t % NXS
                    S.dma("sync", lambda e, t=t, xs_i=xs_i: e.dma_start(out=xt[xs_i][:], in_=xpre_d[t * 128:(t + 1) * 128, :]),
                          writes=[b_xt[xs_i]])
                    ts = NT + t % 2
                    xi = t % 2
                    rms_stats(ts, xt[xs_i][:], b_xt[xs_i], 0, xn[xi], b_xn[xi])
                    S.op("vector", lambda e, ts=ts, xs_i=xs_i, xi=xi: e.tensor_scalar(out=xn[xi][:], in0=xt[xs_i][:],
                                                                                     scalar1=stat[:, ts, 2:3], scalar2=None,
                                                                                     op0=ALU.mult),
                         reads=[b_xt[xs_i], b_stat[ts]], writes=[b_xn[xi]])

                    def tr(e, xi=xi):
                        ins = None
                        for k in range(8):
                            ins = e.transpose(out=pT[:, k * 128:(k + 1) * 128], in_=xn[xi][:, k * 128:(k + 1) * 128],
                                              identity=identb[:])
                        return ins
                    S.op("tensor", tr, reads=[b_xn[xi], b_identb], writes=[b_pT])
                    S.op("vector", lambda e, nb=nb, j=j: e.tensor_copy(
                        out=n1T[nb][:, :, j * 128:(j + 1) * 128], in_=pT[:].rearrange("p (k t) -> p k t", t=128)),
                        reads=[b_pT], writes=[b_n1T[nb][j]])
                emit_precast(1 + (pst % 2), b_n1T[nb][0])
                for j in range(NJ):
                    def ip(e, j=j, nb=nb):
                        ins = None
                        for k in range(8):
                            ins = e.matmul(pG[:], lhsT=n1T[nb][:, k, j * 128:(j + 1) * 128], rhs=win_bf[:, k, 2048:2560],
                                           start=(k == 0), stop=(k == 7))
                        return ins
                    S.op("tensor", ip, reads=b_win + [b_n1T[nb][j]], writes=[b_pG])
                    S.op("scalar", lambda e, j=j, i2=i2: e.activation(out=vip[i2][:, j, :], in_=pG[:], func=AF.Copy),
                         reads=[b_pG], writes=[b_vip[i2]])
                for bk in range(2):
                    def fp(e, bk=bk, nb=nb):
                        ins = None
                        for hh in range(2):
                            h = bk * 2 + hh
                            for k in range(8):
                                ins = e.matmul(pP[bk][:, hh * ST:(hh + 1) * ST], lhsT=win_bf[:, k, 1536 + h * 128:1536 + (h + 1) * 128],
                                               rhs=n1T[nb][:, k, :], start=(k == 0), stop=(k == 7))
                        return ins
                    S.op("tensor", fp, reads=b_win + b_n1T[nb], writes=[b_pP[bk]])
                    S.op("scalar", lambda e, bk=bk, i2=i2: e.activation(
                        out=sg4[i2][:, bk * 2:(bk + 1) * 2, :], in_=pP[bk][:, 0:2 * ST].rearrange("p (h t) -> p h t", t=ST), func=AF.Exp,
                        scale=-1.0), reads=[b_pP[bk]], writes=[b_sg4[i2]])
                S.op("scalar", lambda e, i2=i2: e.activation(out=sg4[i2][:], in_=sg4[i2][:], func=AF.Ln, bias=oneb[:, 0:1]),
                     reads=[b_sg4[i2]], writes=[b_sg4[i2]])
                S.op("scalar", lambda e, i2=i2: e.activation(out=sg4[i2][:], in_=sg4[i2][:], func=AF.Exp, scale=-1.0),
                     reads=[b_sg4[i2]], writes=[b_sg4[i2]])

            banks3 = ((pS, b_pS), (pO, b_pO), (pU, b_pU))
            bank_ctr = [0]

            def pre_B(pst, i2, last):
                for h in range(4):
                    S.op("scalar", lambda e, h=h, i2=i2: e.activation(out=l14[:, h, :], in_=sg4[i2][:, h, :], func=AF.Ln,
                                                                       scale=lbt[:, 4 + h:5 + h], bias=lbt[:, h:h + 1]),
                         reads=[b_sg4[i2], b_lbt], writes=[b_l14])
                for h in range(4):
                    S.op("vector", lambda e, i2=i2, h=h: e.tensor_scalar(
                        out=k4[:, h, :], in0=sg4[i2][:, h, :], scalar1=lbt[:, 8 + h:9 + h], scalar2=lbt[:, 4 + h:5 + h],
                        op0=ALU.mult, op1=ALU.add), reads=[b_sg4[i2], b_lbt], writes=[b_k4])
                S.op("vector", lambda e: e.tensor_tensor_scan(out=bT4[:].rearrange("p h t -> p (h t)"), data0=rmask[:],
                                                              data1=l14[:].rearrange("p h t -> p (h t)"), initial=0.0,
                                                              op0=ALU.mult, op1=ALU.add),
                     reads=[b_l14, b_rmask], writes=[b_bT4])
                bv4 = bT4[:].rearrange("p h (c t) -> p (h c) t", t=64)
                S.op("vector", lambda e: e.tensor_tensor(
                    out=d34[:].rearrange("p h (c t) -> p (h c) t", t=64), in0=bv4,
                    in1=bv4[:, :, 63:64].to_broadcast([128, 4 * NCH, 64]), op=ALU.subtract),
                    reads=[b_bT4], writes=[b_d34])
                S.op("vector", lambda e: e.tensor_copy(out=blp[:].rearrange("p h c -> p (h c)"), in_=bv4[:, :, 63]),
                     reads=[b_bT4], writes=[b_blp])
                S.op("scalar", lambda e: e.activation(out=d34[:], in_=d34[:], func=AF.Exp, scale=-1.0), reads=[b_d34], writes=[b_d34])
                S.op("scalar", lambda e: e.activation(out=decp[:], in_=blp[:], func=AF.Exp), reads=[b_blp], writes=[b_decp])
                S.op("gpsimd", lambda e: e.tensor_tensor(out=kd4[:], in0=k4[:], in1=d34[:], op=ALU.mult),
                     reads=[b_k4, b_d34], writes=[b_kd4])

                def trk(e):
                    ins = None
                    for h in range(4):
                        for j in range(NJ):
                            c0_ = (h * NJ + j) * 128
                            ins = e.transpose(out=pK[:, c0_:c0_ + 128], in_=kd4[:, h, j * 128:(j + 1) * 128], identity=identb[:])
                    return ins
                S.op("tensor", trk, reads=[b_kd4, b_identb], writes=b_pK)
                S.op("scalar", lambda e: e.activation(
                    out=kdtm[0][0:64, :, :, :], in_=pK[0:64, 0:4 * NJ * 128].rearrange("p (h j d) -> p h j d", j=NJ, d=128), func=AF.Copy),
                    reads=b_pK, writes=b_kdtm)
                S.op("scalar", lambda e: e.activation(
                    out=kdtm[1][64:128, :, :, :], in_=pK[64:128, 0:4 * NJ * 128].rearrange("p (h j d) -> p h j d", j=NJ, d=128), func=AF.Copy),
                    reads=b_pK, writes=b_kdtm)
                for j in range(NJ):
                    for half in range(2):
                        c = j * 2 + half
                        bank, b_bank = banks3[bank_ctr[0] % 3]
                        bank_ctr[0] += 1

                        def um(e, j=j, half=half, bank=bank, i2=i2):
                            ins = None
                            for h in range(4):
                                ins = e.matmul(bank[:, h * 128:(h + 1) * 128], lhsT=kdtm[half][:, h, j, :],
                                               rhs=vip[i2][:, j, h * 128:(h + 1) * 128], start=True, stop=True)
                            return ins
                        S.op("tensor", um, reads=b_kdtm + [b_vip[i2]], writes=b_bank)
                        for h in range(4):
                            S.op("vector", lambda e, c=c, h=h, bank=bank: e.scalar_tensor_tensor(
                                out=S32[:, h, :], in0=S32[:, h, :], scalar=decp[:, h, c:c + 1], in1=bank[:, h * 128:(h + 1) * 128],
                                op0=ALU.mult, op1=ALU.add), reads=[b_S32[h], b_decp] + b_bank, writes=[b_S32[h]])
                if last:
                    S.op("scalar", lambda e: e.activation(out=Sbf4[0][:], in_=S32[:], func=AF.Copy),
                         reads=b_S32, writes=[b_Sbf4[0]])

            kpre = int(os.environ.get("KPRE", NPRE))
            plist = list(range(NPRE - kpre, NPRE))
            for n, pst in enumerate(plist):
                pre_A(pst, n % 2)
                if n > 0:
                    pre_B(plist[n - 1], (n - 1) % 2, False)
            if plist:
                pre_B(plist[-1], (len(plist) - 1) % 2, True)
            with nc.Block() as block:
                S.emit(block)
            s1a.close()
            Buf.reset_all()
            o32 = sb(s1, "o32", [128, 4, ST], F32)
            qs4 = sb(s1, "qs4", [128, 4, ST], F32)
            sg4m = sb(s1, "sg4m", [128, 4, ST], F32)
            l14m = sb(s1, "l14m", [128, 4, ST], F32)
            k4m = sb(s1, "k4m", [128, 4, ST], F32)
            bT4m = sb(s1, "bT4m", [128, 4, ST], F32)
            d34m = sb(s1, "d34m", [128, 4, ST], F32)
            E12m = sb(s1, "E12m", [128, 4, ST], F32)
            kd4m = sb(s1, "kd4m", [128, 4, ST], BF16)
            osq4 = sb(s1, "osq4", [128, 4, ST], BF16)
            blm = sb(s1, "blm", [128, 4, NCH], F32)
            decm = sb(s1, "decm", [128, 4, NCH], F32)
            scb4 = [sb(s1, f"scb4{i}", [128, 4, 128], BF16) for i in range(2)]
            b_qs4 = B("qs4"); b_sg4m = B("sg4m"); b_l14m = B("l14m"); b_k4m = B("k4m"); b_bT4m = B("bT4m"); b_d34m = B("d34m")
            b_E12m = B("E12m"); b_kd4m = B("kd4m"); b_osq4 = B("osq4"); b_blm = B("blm"); b_decm = B("decm")
            b_scb4 = [B("scb40"), B("scb41")]
            abT2 = [sb(s1, f"abT{i}", [128, 8, ST], BF16) for i in range(2)]
            b_abT2 = [[[B(f"abT{i}_{c}_{j}") for j in range(NJ)] for c in range(8)] for i in range(2)]
            sgT2 = [sgT, sb(s1, "sgTb", [128, 4, ST], BF16)]
            b_sgT2 = [b_sgT, [B(f"sgTb{g}") for g in range(4)]]
            hx = [sb(s1, f"hx{i}", [128, D], F32) for i in range(2)]
            n2f = sb(s1, "n2f", [128, D], F32)
            n2T = sb(s1, "n2T", [128, 8, 128], F32)
            S = Sched(nc, s1, "m")
            gcount[0] = 0
            for st in range(NST):
                super_tile(st, False)
            emit_precast(len(precast), b_lg)
            with nc.Block() as block:
                S.emit(block)

        with ExitStack() as s2:
            if phases < 2:
                return nc
            S = Sched(nc, s2, "b")
            B = Buf
            gfbc = sb(s2, "gfbc", [128, D], F32)
            blkthr = sb(s2, "blkthr", [128, NB], F32)
            onesb2 = sb(s2, "onesb2", [128, 128], BF16)
            trib = sb(s2, "trib", [128, 128], BF16)
            identb2 = sb(s2, "identb2", [128, 128], BF16)
            R = sb(s2, "R", [128, 24, NT], F32)
            ohg = sb(s2, "ohg", [128, NT, 4], F32)
            gex = sb(s2, "gex", [128, NT, 4], F32)
            pen = sb(s2, "pen", [128, NT, 32], F32)
            elm = sb(s2, "elm", [128, NT, 32], F32)
            elm2 = sb(s2, "elm2", [128, NT, 32], F32)
            oh1 = sb(s2, "oh1", [128, NT, 32], F32)
            oh2 = sb(s2, "oh2", [128, NT, 32], F32)
            ohb = sb(s2, "ohb", [128, NT, 32], BF16)
            cum = sb(s2, "cum", [128, NT, 32], F32)
            prod = sb(s2, "prod", [128, NT, 32], F32)
            cnt = sb(s2, "cnt", [128, 6, 32], F32)
            ebt = sb(s2, "ebt", [128, 2, NB], F32)
            widx = [sb(s2, f"widx{b}", [128, 1], I32) for b in range(NB)]
            dst = [[sb(s2, f"dst{k}_{t}", [128, 1], I32) for t in range(NT)] for k in range(2)]
            NR = 6
            n2r = [sb(s2, f"n2r{i}", [128, D], BF16) for i in range(NR)]
            NWB = 3
            wbf = [[sb(s2, f"wbf{i}_{p}", [128, 4096], BF16) for p in range(3)] for i in range(NWB)]
            xb = [sb(s2, f"xb{i}", [128, 2, D], BF16) for i in range(2)]
            xT = [sb(s2, f"xT{i}", [128, 8, BLK], BF16) for i in range(2)]
            sg = [sb(s2, f"sg{i}", [128, BLK], F32) for i in range(2)]
            hT = [sb(s2, f"hT{i}", [128, 4, BLK], BF16) for i in range(2)]
            yo = [sb(s2, f"yo{i}", [128, D], F32) for i in range(2)]
            hq = [sb(s2, f"hq{i}", [128, D], F32) for i in range(3)]
            y1 = [sb(s2, f"y1{i}", [128, D], F32) for i in range(3)]
            y2 = [sb(s2, f"y2{i}", [128, D], F32) for i in range(3)]
            sq2 = sb(s2, "sq2", [128, D], BF16)
            fst = sb(s2, "fst", [128, NT, 4], F32)
            ot = [sb(s2, f"ot{i}", [128, D], F32) for i in range(3)]
            pC = ps(s2, "pC", [128, 512], F32)
            pTot = ps(s2, "pTot", [128, 512], F32)
            pX = [ps(s2, f"pX{i}", [128, 1024], BF16) for i in range(2)]
            pGt = [ps(s2, f"pGt{i}", [128, 512], F32) for i in range(2)]
            pY = [ps(s2, f"pY{i}", [128, 512], F32) for i in range(2)]

            b_cst = B("cst"); b_small = B("small"); b_lg = B("lg")
            b_gfbc = B("gfbc"); b_blkthr = B("blkthr"); b_onesb2 = B("onesb2"); b_trib = B("trib"); b_identb2 = B("identb2")
            b_R = B("R"); b_ohg = B("ohg"); b_gex = B("gex"); b_pen = B("pen"); b_elm = B("elm"); b_elm2 = B("elm2")
            b_oh1 = B("oh1"); b_oh2 = B("oh2"); b_ohb = B("ohb"); b_cum = B("cum"); b_prod = B("prod"); b_cnt = B("cnt")
            b_ebt = B("ebt")
            b_widx = [B(f"widx{b}") for b in range(NB)]
            b_dst = [[B(f"dst{k}_{t}") for t in range(NT)] for k in range(2)]
            b_n2r = [B(f"n2r{i}") for i in range(NR)]
            b_wbf = [[B(f"wbf{i}_{p}") for p in range(3)] for i in range(NWB)]
            b_xb = [B(f"xb{i}") for i in range(2)]
            b_xT = [B(f"xT{i}") for i in range(2)]
            b_sg = [B(f"sg{i}") for i in range(2)]
            b_hT = [B(f"hT{i}") for i in range(2)]
            b_yo = [B(f"yo{i}") for i in range(2)]
            b_hq = [B(f"hq{i}") for i in range(3)]
            b_y1 = [B(f"y1{i}") for i in range(3)]
            b_y2 = [B(f"y2{i}") for i in range(3)]
            b_sq2 = B("sq2")
            b_fst = [B(f"fst{t}") for t in range(NT)]
            b_ot = [B(f"ot{i}") for i in range(3)]
            b_pC = B("pC"); b_pTot = B("pTot")
            b_pX = [B("pX0"), B("pX1")]
            b_pGt = [B("pGt0"), B("pGt1")]
            b_pY = [B("pY0"), B("pY1")]
            b_xsl = [B(f"xs_{i}") for i in range(2 * NT)]
            b_ysl = [B(f"ys_{i}") for i in range(2 * NB)]

            S.dma("sync", lambda e: e.dma_start(out=gfbc[:], in_=gfbc_d[:, :]), writes=[b_gfbc])
            S.dma("sync", lambda e: e.dma_start(out=blkthr[:], in_=blkthr_d[:, :]), writes=[b_blkthr])
            S.op("vector", lambda e: e.tensor_copy(out=identb2[:], in_=cst[:, 0, :]), reads=[b_cst], writes=[b_identb2])
            S.op("vector", lambda e: e.tensor_copy(out=trib[:], in_=cst[:, 3, :]), reads=[b_cst], writes=[b_trib])
            S.op("vector", lambda e: e.tensor_copy(out=onesb2[:], in_=cst[:, 4, :]), reads=[b_cst], writes=[b_onesb2])

            gl = lg[:, :, 0:4]
            el = lg[:, :, 4:36]
            V = S.op
            V("vector", lambda e: e.tensor_reduce(out=R[:, 0, :], in_=gl, axis=AX.X, op=ALU.max), reads=[b_lg], writes=[b_R])
            V("vector", lambda e: e.tensor_tensor(out=ohg[:], in0=gl, in1=R[:, 0, :].rearrange("p (t o) -> p t o", o=1).to_broadcast([128, NT, 4]),
                                                  op=ALU.is_equal), reads=[b_lg, b_R], writes=[b_ohg])
            V("vector", lambda e: e.tensor_tensor(out=gex[:], in0=gl, in1=R[:, 0, :].rearrange("p (t o) -> p t o", o=1).to_broadcast([128, NT, 4]),
                                                  op=ALU.subtract), reads=[b_lg, b_R], writes=[b_gex])
            V("scalar", lambda e: e.activation(out=gex[:], in_=gex[:], func=AF.Exp), reads=[b_gex], writes=[b_gex])
            V("vector", lambda e: e.tensor_reduce(out=R[:, 1, :], in_=gex[:], axis=AX.X, op=ALU.add), reads=[b_gex], writes=[b_R])
            V("vector", lambda e: e.reciprocal(out=R[:, 2, :], in_=R[:, 1, :]), reads=[b_R], writes=[b_R])
            V("vector", lambda e: e.tensor_scalar(
                out=pen[:].rearrange("p t (g j) -> p t g j", j=8),
                in0=ohg[:].rearrange("p t (g o) -> p t g o", o=1).to_broadcast([128, NT, 4, 8]),
                scalar1=-1.0, scalar2=BIG, op0=ALU.add, op1=ALU.mult), reads=[b_ohg], writes=[b_pen])
            V("vector", lambda e: e.tensor_tensor(out=elm[:], in0=el, in1=pen[:], op=ALU.add), reads=[b_lg, b_pen], writes=[b_elm])
            V("vector", lambda e: e.tensor_reduce(out=R[:, 3, :], in_=elm[:], axis=AX.X, op=ALU.max), reads=[b_elm], writes=[b_R])
            V("vector", lambda e: e.tensor_tensor(out=oh1[:], in0=elm[:], in1=R[:, 3, :].rearrange("p (t o) -> p t o", o=1).to_broadcast([128, NT, 32]),
                                                  op=ALU.is_equal), reads=[b_elm, b_R], writes=[b_oh1])
            V("vector", lambda e: e.scalar_tensor_tensor(out=elm2[:], in0=oh1[:], scalar=-BIG, in1=elm[:], op0=ALU.mult, op1=ALU.add),
              reads=[b_oh1, b_elm], writes=[b_elm2])
            V("vector", lambda e: e.tensor_reduce(out=R[:, 4, :], in_=elm2[:], axis=AX.X, op=ALU.max), reads=[b_elm2], writes=[b_R])
            V("vector", lambda e: e.tensor_tensor(out=oh2[:], in0=elm2[:], in1=R[:, 4, :].rearrange("p (t o) -> p t o", o=1).to_broadcast([128, NT, 32]),
                                                  op=ALU.is_equal), reads=[b_elm2, b_R], writes=[b_oh2])
            V("vector", lambda e: e.tensor_tensor(out=R[:, 5, :], in0=R[:, 4, :], in1=R[:, 3, :], op=ALU.subtract), reads=[b_R], writes=[b_R])
            V("scalar", lambda e: e.activation(out=R[:, 5, :], in_=R[:, 5, :], func=AF.Exp), reads=[b_R], writes=[b_R])
            V("vector", lambda e: e.tensor_scalar(out=R[:, 5, :], in0=R[:, 5, :], scalar1=1.0, scalar2=None, op0=ALU.add), reads=[b_R], writes=[b_R])
            V("vector", lambda e: e.reciprocal(out=R[:, 6, :], in_=R[:, 5, :]), reads=[b_R], writes=[b_R])
            V("vector", lambda e: e.tensor_scalar(out=R[:, 7, :], in0=R[:, 6, :], scalar1=-1.0, scalar2=1.0, op0=ALU.mult, op1=ALU.add),
              reads=[b_R], writes=[b_R])
            V("vector", lambda e: e.tensor_tensor(out=R[:, 8, :], in0=R[:, 6, :], in1=R[:, 2, :], op=ALU.mult), reads=[b_R], writes=[b_R])
            V("vector", lambda e: e.tensor_tensor(out=R[:, 9, :], in0=R[:, 7, :], in1=R[:, 2, :], op=ALU.mult), reads=[b_R], writes=[b_R])
            V("vector", lambda e: e.tensor_tensor(out=ohb[:], in0=oh1[:], in1=oh2[:], op=ALU.add), reads=[b_oh1, b_oh2], writes=[b_ohb])

            def cumf(e):
                ins = None
                for i in range(NT):
                    for i2 in range(i):
                        e.matmul(pC[:, i * 32:(i + 1) * 32], lhsT=onesb2[:], rhs=ohb[:, i2, :], start=(i2 == 0), stop=False)
                    ins = e.matmul(pC[:, i * 32:(i + 1) * 32], lhsT=trib[:], rhs=ohb[:, i, :], start=(i == 0), stop=True)
                return ins
            V("tensor", cumf, reads=[b_ohb, b_onesb2, b_trib], writes=[b_pC])

            def totf(e):
                ins = None
                for i in range(NT):
                    ins = e.matmul(pTot[:, 0:32], lhsT=onesb2[:], rhs=ohb[:, i, :], start=(i == 0), stop=(i == NT - 1))
                return ins
            V("tensor", totf, reads=[b_ohb, b_onesb2], writes=[b_pTot])
            V("vector", lambda e: e.tensor_copy(out=cum[:], in_=pC[:].rearrange("p (t x) -> p t x", x=32)), reads=[b_pC], writes=[b_cum])
            V("vector", lambda e: e.tensor_copy(out=cnt[:, 0, :], in_=pTot[:, 0:32]), reads=[b_pTot], writes=[b_cnt])
            V("vector", lambda e: e.memset(cnt[:, 1, :], 0.0), reads=[b_cnt], writes=[b_cnt])
            for m in range(TOK // BLK):
                V("vector", lambda e, m=m: e.scalar_tensor_tensor(out=cnt[:, 1, :], in0=cnt[:, 0, :], scalar=float(m * BLK) + 0.5,
                                                                  in1=cnt[:, 1, :], op0=ALU.is_gt, op1=ALU.add),
                  reads=[b_cnt], writes=[b_cnt])
            V("vector", lambda e: e.tensor_scalar(out=cnt[:, 2, :], in0=cnt[:, 1, :], scalar1=float(BLK), scalar2=None, op0=ALU.mult),
              reads=[b_cnt], writes=[b_cnt])
            V("vector", lambda e: e.memset(cnt[:, 5, :], 1.0), reads=[b_cnt], writes=[b_cnt])
            V("vector", lambda e: e.tensor_tensor_scan(out=cnt[:, 3, :], data0=cnt[:, 5, :], data1=cnt[:, 2, :], initial=0.0,
                                                       op0=ALU.mult, op1=ALU.add), reads=[b_cnt], writes=[b_cnt])
            V("vector", lambda e: e.tensor_tensor(out=cnt[:, 4, :], in0=cnt[:, 3, :], in1=cnt[:, 2, :], op=ALU.subtract),
              reads=[b_cnt], writes=[b_cnt])
            V("vector", lambda e: e.tensor_tensor(out=cum[:], in0=cum[:], in1=cnt[:, 4:5, :].to_broadcast([128, NT, 32]), op=ALU.add),
              reads=[b_cum, b_cnt], writes=[b_cum])
            for k, ohk in ((0, oh1), (1, oh2)):
                V("vector", lambda e, ohk=ohk: e.tensor_tensor(out=prod[:], in0=cum[:], in1=ohk[:], op=ALU.mult),
                  reads=[b_cum, b_oh1, b_oh2], writes=[b_prod])
                V("vector", lambda e, k=k: e.tensor_reduce(out=R[:, 10 + k, :], in_=prod[:], axis=AX.X, op=ALU.add),
                  reads=[b_prod], writes=[b_R])
                for t in range(NT):
                    V("vector", lambda e, k=k, t=t: e.tensor_copy(out=dst[k][t][:], in_=R[:, 10 + k, t:t + 1]),
                      reads=[b_R], writes=[b_dst[k][t]])
            V("vector", lambda e: e.memset(ebt[:, 0, :], 0.0), writes=[b_ebt])
            for ex in range(32):
                V("vector", lambda e, ex=ex: e.scalar_tensor_tensor(out=ebt[:, 0, :], in0=blkthr[:], scalar=cnt[:, 3, ex:ex + 1],
                                                                    in1=ebt[:, 0, :], op0=ALU.is_ge, op1=ALU.add),
                  reads=[b_blkthr, b_cnt, b_ebt], writes=[b_ebt])
            V("vector", lambda e: e.tensor_scalar(out=ebt[:, 0, :], in0=ebt[:, 0, :], scalar1=128.0, scalar2=None, op0=ALU.mult),
              reads=[b_ebt], writes=[b_ebt])
            V("vector", lambda e: e.tensor_scalar(out=ebt[:, 1, :], in0=ebt[:, 0, :], scalar1=small[:, 24:25], scalar2=None, op0=ALU.add),
              reads=[b_ebt, b_small], writes=[b_ebt])
            for b in range(NB):
                V("vector", lambda e, b=b: e.tensor_copy(out=widx[b][:], in_=ebt[:, 1, b:b + 1]), reads=[b_ebt], writes=[b_widx[b]])
            if debug:
                S.dma("sync", lambda e: e.dma_start(out=dbg_lg[:, :, :], in_=lg[:]), reads=[b_lg], writes=[B("dl")], key="dl", final=True)
                S.dma("sync", lambda e: e.dma_start(out=dbg_r[:, :, :], in_=R[:, 4:12, :]), reads=[b_R] + b_dst[1], writes=[B("dr")], key="dr", final=True)

            for t in range(NT):
                i = t % NR
                S.dma("sync", lambda e, t=t, i=i: e.dma_start(out=n2r[i][:], in_=n2s_d[t * 128:(t + 1) * 128, :]), writes=[b_n2r[i]])
                for k in range(2):
                    S.dma("gpsimd", lambda e, t=t, i=i, k=k: e.indirect_dma_start(
                        out=xs_d[:, :], out_offset=bass.IndirectOffsetOnAxis(ap=dst[k][t][:, :], axis=0),
                        in_=n2r[i][:], in_offset=None, bounds_check=_bc(e, NSLOT - 1), oob_is_err=False),
                        reads=[b_n2r[i], b_dst[k][t]], writes=[b_xsl[2 * t + k]], key=f"xs{i}")

            for b in range(NB):
                i = b % 2
                wi = b % NWB
                for p, wsrc in enumerate((wgb_d, wub_d, wdb_d)):
                    S.dma("gpsimd", lambda e, b=b, p=p, wsrc=wsrc, wi=wi: e.indirect_dma_start(
                        out=wbf[wi][p][:], out_offset=None, in_=wsrc[:, :],
                        in_offset=bass.IndirectOffsetOnAxis(ap=widx[b][:, :], axis=0), bounds_check=_bc(e, 32 * 128 - 1), oob_is_err=False),
                        reads=[b_widx[b]], writes=[b_wbf[wi][p]])
                S.dma("sync", lambda e, b=b, i=i: e.dma_start(
                    out=xb[i][:], in_=xs_d[b * BLK:(b + 1) * BLK, :].rearrange("(s p) d -> p s d", p=128)),
                    reads=b_xsl, writes=[b_xb[i]])
                for s in range(2):
                    def trx(e, i=i, s=s):
                        ins = None
                        for k in range(8):
                            ins = e.transpose(out=pX[s][:, k * 128:(k + 1) * 128], in_=xb[i][:, s, k * 128:(k + 1) * 128],
                                              identity=identb2[:])
                        return ins
                    S.op("tensor", trx, reads=[b_xb[i], b_identb2], writes=[b_pX[s]])
                    S.op("vector" if s == 0 else "gpsimd" if False else "vector", lambda e, i=i, s=s: e.tensor_copy(
                        out=xT[i][:, :, s * 128:(s + 1) * 128], in_=pX[s][:].rearrange("p (k t) -> p k t", t=128)),
                        reads=[b_pX[s]], writes=[b_xT[i]])
                wgv = wbf[wi][0][:].rearrange("p (k f) -> p k f", f=512)
                wuv = wbf[wi][1][:].rearrange("p (k f) -> p k f", f=512)
                wdv = wbf[wi][2][:].rearrange("p (c d) -> p c d", d=1024)
                for fc in range(4):
                    def gmm(e, wv, pi, fc=fc, i=i):
                        ins = None
                        for k in range(8):
                            ins = e.matmul(pGt[pi][:, 0:BLK], lhsT=wv[:, k, fc * 128:(fc + 1) * 128], rhs=xT[i][:, k, :],
                                           start=(k == 0), stop=(k == 7))
                        return ins
                    S.op("tensor", lambda e, fc=fc, wgv=wgv, i=i, gmm=gmm: gmm(e, wgv, 0, fc, i), reads=[b_wbf[wi][0], b_xT[i]], writes=[b_pGt[0]])
                    S.op("tensor", lambda e, fc=fc, wuv=wuv, i=i, gmm=gmm: gmm(e, wuv, 1, fc, i), reads=[b_wbf[wi][1], b_xT[i]], writes=[b_pGt[1]])
                    si = fc % 2
                    S.op("scalar", lambda e, si=si: e.activation(out=sg[si][:], in_=pGt[0][:, 0:BLK], func=AF.Silu),
                         reads=[b_pGt[0]], writes=[b_sg[si]])
                    S.op("vector", lambda e, si=si, fc=fc, i=i: e.tensor_tensor(out=hT[i][:, fc, :], in0=pGt[1][:, 0:BLK], in1=sg[si][:], op=ALU.mult),
                         reads=[b_pGt[1], b_sg[si]], writes=[b_hT[i]])
                for s in range(2):
                    yi = s
                    for half in range(2):
                        def dmm(e, s=s, half=half, i=i, wdv=wdv):
                            ins = None
                            for fc in range(4):
                                ins = e.matmul(pY[half][:], lhsT=hT[i][:, fc, s * 128:(s + 1) * 128],
                                               rhs=wdv[:, fc, half * 512:(half + 1) * 512], start=(fc == 0), stop=(fc == 3))
                            return ins
                        S.op("tensor", dmm, reads=[b_hT[i], b_wbf[wi][2]], writes=[b_pY[half]])
                        if half == 0:
                            S.op("scalar", lambda e, yi=yi: e.activation(out=yo[yi][:, 0:512], in_=pY[0][:], func=AF.Copy),
                                 reads=[b_pY[0]], writes=[b_yo[yi]])
                        else:
                            S.op("vector", lambda e, yi=yi: e.tensor_copy(out=yo[yi][:, 512:1024], in_=pY[1][:]),
                                 reads=[b_pY[1]], writes=[b_yo[yi]])
                    S.dma("sync", lambda e, b=b, s=s, yi=yi: e.dma_start(out=ys_d[b * BLK + s * 128: b * BLK + (s + 1) * 128, :], in_=yo[yi][:]),
                          reads=[b_yo[yi]], writes=[b_ysl[2 * b + s]], key=f"ys{yi}")

            for t in range(NT):
                i = t % 3
                S.dma("sync", lambda e, t=t, i=i: e.dma_start(out=hq[i][:], in_=hs_d[t * 128:(t + 1) * 128, :]), writes=[b_hq[i]])
                for k, (yk, b_yk) in enumerate(((y1, b_y1), (y2, b_y2))):
                    S.dma("gpsimd", lambda e, t=t, i=i, k=k, yk=yk: e.indirect_dma_start(
                        out=yk[i][:], out_offset=None, in_=ys_d[:, :],
                        in_offset=bass.IndirectOffsetOnAxis(ap=dst[k][t][:, :], axis=0), bounds_check=_bc(e, NSLOT - 1), oob_is_err=False),
                        reads=b_ysl + [b_dst[k][t]], writes=[b_yk[i]])
                S.op("vector", lambda e, t=t, i=i: e.scalar_tensor_tensor(out=hq[i][:], in0=y1[i][:], scalar=R[:, 8, t:t + 1], in1=hq[i][:],
                                                                         op0=ALU.mult, op1=ALU.add),
                     reads=[b_y1[i], b_R, b_hq[i]], writes=[b_hq[i]])
                S.op("vector", lambda e, t=t, i=i: e.scalar_tensor_tensor(out=hq[i][:], in0=y2[i][:], scalar=R[:, 9, t:t + 1], in1=hq[i][:],
                                                                         op0=ALU.mult, op1=ALU.add),
                     reads=[b_y2[i], b_R, b_hq[i]], writes=[b_hq[i]])
                S.op("scalar", lambda e, t=t, i=i: e.activation(out=sq2[:], in_=hq[i][:], func=AF.Square, accum_out=fst[:, t, 0:1]),
                     reads=[b_hq[i]], writes=[b_sq2, b_fst[t]])
                S.op("scalar", lambda e, t=t: e.activation(out=fst[:, t, 1:2], in_=fst[:, t, 0:1], func=AF.Ln, scale=1.0 / D, bias=epsb[:, 0:1]),
                     reads=[b_fst[t]], writes=[b_fst[t]])
                S.op("scalar", lambda e, t=t: e.activation(out=fst[:, t, 2:3], in_=fst[:, t, 1:2], func=AF.Exp, scale=-0.5),
                     reads=[b_fst[t]], writes=[b_fst[t]])
                S.op("vector", lambda e, t=t, i=i: e.scalar_tensor_tensor(out=ot[i][:], in0=hq[i][:], scalar=fst[:, t, 2:3], in1=gfbc[:],
                                                                         op0=ALU.mult, op1=ALU.mult),
                     reads=[b_hq[i], b_fst[t], b_gfbc], writes=[b_ot[i]])
                S.dma("sync", lambda e, t=t, i=i: e.dma_start(out=out_d[t * 128:(t + 1) * 128, :], in_=ot[i][:]),
                      reads=[b_ot[i]], writes=[B(f"out_t{t}")], key=f"out{i}", final=True)
            with nc.Block() as block:
                S.emit(block)
    return nc


def _host_inputs(inp):
    f = np.float32
    w_in = np.asarray(inp["w_in"][0], f)
    win = np.ascontiguousarray(w_in.reshape(8, 128, 3072).transpose(1, 0, 2))
    wout = np.ascontiguousarray(np.asarray(inp["w_out"][0], f).reshape(8, 128, 1024).transpose(1, 0, 2))
    wsT = np.ascontiguousarray(np.asarray(inp["gmlp_w_s"][0], f).transpose(2, 0, 1))
    bsbc = np.ascontiguousarray(np.broadcast_to(np.asarray(inp["gmlp_b_s"][0], f)[None], (128, 4, 128)))
    small = np.zeros((128, 32), f)
    small[:, 0:4] = np.asarray(inp["gmlp_v_gain"][0], f).reshape(4, 128).T
    lbr = np.asarray(inp["hgrn_lower_bounds"], f)
    small[:, 4:12] = lbr.reshape(2, 4, 128).transpose(2, 1, 0).reshape(128, 8)
    small[:, 12:16] = np.asarray(inp["hgrn_out_gain"][0], f).reshape(4, 128).T
    small[:, 16:24] = np.asarray(inp["norm1_gain"][0], f).reshape(8, 128).T
    small[:, 24] = np.arange(128, dtype=f)
    g2bc = np.ascontiguousarray(np.broadcast_to(np.asarray(inp["norm2_gain"][0], f)[None], (128, D)))
    gfbc = np.ascontiguousarray(np.broadcast_to(np.asarray(inp["final_gain"], f)[None], (128, D)))
    wrf = np.concatenate([np.asarray(inp["w_group_router"][0], f), np.asarray(inp["w_expert_router"][0], f)], axis=1)
    wr = np.ascontiguousarray(wrf.reshape(8, 128, 36).transpose(1, 0, 2))
    br = np.concatenate([np.asarray(inp["b_group_router"][0], f), np.asarray(inp["b_expert_router"][0], f)])
    brbc = np.ascontiguousarray(np.broadcast_to(br[None], (128, 36)))
    idx = np.arange(128)
    consts = np.zeros((128, 5, 128), f)
    consts[:, 0, :] = np.eye(128, dtype=f)
    consts[:, 1, :] = ((idx[:, None] // 64 == idx[None, :] // 64) & (idx[:, None] <= idx[None, :])).astype(f)
    consts[:, 2, :] = (idx[:, None] // 64 <= idx[None, :] // 64).astype(f)
    consts[:, 3, :] = (idx[:, None] < idx[None, :]).astype(f)
    consts[:, 4, :] = 1.0
    rmask = np.ones((128, 1024), f)
    rmask[:, 0::64] = 0.0
    blkthr = np.ascontiguousarray(np.broadcast_to((np.arange(NB, dtype=f) * BLK)[None], (128, NB)))
    wg = np.ascontiguousarray(np.asarray(inp["w_gate"][0], f).reshape(32, 8, 128, 512).transpose(0, 2, 1, 3)).reshape(32 * 128, 4096)
    wu = np.ascontiguousarray(np.asarray(inp["w_up"][0], f).reshape(32, 8, 128, 512).transpose(0, 2, 1, 3)).reshape(32 * 128, 4096)
    wd = np.ascontiguousarray(np.asarray(inp["w_down"][0], f).reshape(32, 4, 128, 1024).transpose(0, 2, 1, 3)).reshape(32 * 128, 4096)
    return dict(win=win, wout=wout, wsT=wsT, bsbc=bsbc, small=small, g2bc=g2bc, gfbc=gfbc, wr=wr, brbc=brbc,
                consts=consts, rmask=rmask, blkthr=blkthr, wg=wg, wu=wu, wd=wd)


def _prefix(x, c):
    p = c % 4
    pre = np.zeros((3 * TOK, D), np.float32)
    if p > 0:
        prev = x[c - p:c].reshape(p * TOK, D)
        pre[(3 - p) * TOK:] = prev
    return pre


_NC_CACHE = {}


def kernel(**inputs):
    x = np.asarray(inputs["x"], np.float32).reshape(NCORES, TOK, D)
    shared = _host_inputs(inputs)
    if "nc" not in _NC_CACHE:
        _NC_CACHE["nc"] = build(phases=int(os.environ.get("KPH", "2")))
    nc = _NC_CACHE["nc"]
    in_maps = []
    for c in range(NCORES):
        m = dict(shared)
        if int(os.environ.get("KPH", "2")) < 2:
            for kk in ("wg", "wu", "wd"):
                m.pop(kk, None)
        m["x"] = np.ascontiguousarray(x[c])
        m["xpre"] = _prefix(x, c)
        in_maps.append(m)
    res = run_bass_kernel_spmd(nc, in_maps, core_ids=list(range(NCORES)))
    out = np.stack([np.asarray(r["out"], np.float32) for r in res.results], axis=0)
    return out.reshape(2, 8192, D)
```
